# Optimizing a Trainium2 kernel written in Bass

```python
import math
import jax, jax.numpy as jnp
from jax import lax
import numpy as np

D_MODEL = 1024
BATCH = 4
SEQ = 8192
DEPTH = 2

GLA_HEADS = 4
GLA_DV = 96
GLA_DK = 48
GLA_QK = GLA_HEADS * GLA_DK
GLA_WIDTH = GLA_HEADS * GLA_DV
GLA_RANK = 16
GLA_TAU = 16.0
GLA_CHUNK = 64
DSA_HEADS = 4
DSA_HEAD_DIM = 64
DSA_WIDTH = DSA_HEADS * DSA_HEAD_DIM
DSA_IDX_HEADS = 8
DSA_IDX_DIM = 32
DSA_TOPK = 256
DSA_QBLOCK = 128
S5_GROUPS = 24
S5_GROUP_CH = 16
S5_STATE = 64
S5_WIDTH = S5_GROUPS * S5_GROUP_CH
D_MIX = GLA_WIDTH + DSA_WIDTH + S5_WIDTH
IN_SIZES = (GLA_QK, GLA_QK, GLA_WIDTH, GLA_RANK, GLA_WIDTH,
            DSA_WIDTH, DSA_WIDTH, DSA_WIDTH, DSA_IDX_HEADS * DSA_IDX_DIM, DSA_IDX_DIM, DSA_IDX_HEADS,
            S5_WIDTH)
D_IN = 2616
MOE_GROUPS = 4
MOE_EXPERTS_PER_GROUP = 8
MOE_EXPERTS = MOE_GROUPS * MOE_EXPERTS_PER_GROUP
MOE_TOPK = 2
MOE_HIDDEN = 512
MOE_BLOCK = 128
RMS_EPS = 1e-6

kernel_name = 'hybrid_gla_dsa_s5_hier_moe_block'


def rms_norm(x, g):
    xf = x.astype(jnp.float32)
    y = xf * lax.rsqrt(jnp.mean(xf * xf, axis=-1, keepdims=True) + RMS_EPS)
    return (y * g.astype(jnp.float32)).astype(x.dtype)


def gla_mixer(q, k, v, lr, r, gate_w, gate_b, norm_g):
    B, L, _ = q.shape
    n = L // GLA_CHUNK
    f32 = jnp.float32
    g = jax.nn.log_sigmoid((lr @ gate_w + gate_b).astype(f32)) / GLA_TAU

    def heads(t, d):
        return t.astype(f32).reshape(B, n, GLA_CHUNK, GLA_HEADS, d).transpose(0, 3, 1, 2, 4)

    qh = heads(q, GLA_DK) * GLA_DK ** -0.5
    kh = heads(k, GLA_DK)
    vh = heads(v, GLA_DV)
    bcum = jnp.cumsum(heads(g, GLA_DK), axis=3)
    blast = bcum[:, :, :, -1:, :]
    q_dec = qh * jnp.exp(bcum)
    causal = jnp.tril(jnp.ones((GLA_CHUNK, GLA_CHUNK), dtype=bool))
    att = jnp.einsum('bhncd,bhnsd->bhncs', q_dec, kh * jnp.exp(-bcum))
    att = jnp.where(causal, att, 0.0)
    o_intra = jnp.einsum('bhncs,bhnse->bhnce', att, vh)
    upd = jnp.einsum('bhncd,bhnce->bhnde', kh * jnp.exp(blast - bcum), vh)
    decay = jnp.exp(blast[:, :, :, 0, :])

    def step(S, inp):
        dec, u = inp
        return dec[..., None] * S + u, S

    S0 = jnp.zeros((B, GLA_HEADS, GLA_DK, GLA_DV), f32)
    _, S_in = lax.scan(step, S0, (jnp.moveaxis(decay, 2, 0), jnp.moveaxis(upd, 2, 0)))
    S_in = jnp.moveaxis(S_in, 0, 2)
    o = o_intra + jnp.einsum('bhncd,bhnde->bhnce', q_dec, S_in)
    o = o.transpose(0, 2, 3, 1, 4).reshape(B, L, GLA_HEADS, GLA_DV)
    o = rms_norm(o, norm_g).reshape(B, L, GLA_WIDTH) * jax.nn.silu(r.astype(f32))
    return o.astype(q.dtype)


def dsa_mixer(q, k, v, iq, ik, iw, norm_g):
    B, L, _ = q.shape
    f32 = jnp.float32
    n_sel = min(DSA_TOPK, L // 4)
    nb = L // DSA_QBLOCK
    qh = q.reshape(B, L, DSA_HEADS, DSA_HEAD_DIM)
    kh = k.reshape(B, L, DSA_HEADS, DSA_HEAD_DIM)
    vh = v.reshape(B, L, DSA_HEADS, DSA_HEAD_DIM)
    iqh = iq.reshape(B, L, DSA_IDX_HEADS, DSA_IDX_DIM)
    iwh = iw.astype(f32) * DSA_IDX_HEADS ** -0.5
    ikf = ik.astype(f32)
    key_pos = jnp.arange(L)

    def blocks(t):
        return jnp.moveaxis(t.reshape((B, nb, DSA_QBLOCK) + t.shape[2:]), 1, 0)

    def one_block(inp):
        qb, iqb, iwb, qpos = inp
        logits = jnp.einsum('bqhd,bsd->bqhs', iqb.astype(f32), ikf) * DSA_IDX_DIM ** -0.5
        score = jnp.einsum('bqh,bqhs->bqs', iwb, jax.nn.relu(logits))
        admissible = key_pos[None, :] <= qpos[:, None]
        score = jnp.where(admissible[None], score, -jnp.inf)
        _, sel = lax.top_k(score, n_sel)
        valid = sel <= qpos[None, :, None]
        kg = jax.vmap(lambda kb, ib: kb[ib])(kh, sel)
        vg = jax.vmap(lambda vb, ib: vb[ib])(vh, sel)
        s = jnp.einsum('bqhd,bqkhd->bhqk', qb.astype(f32), kg.astype(f32)) * DSA_HEAD_DIM ** -0.5
        s = jnp.where(valid[:, None], s, -jnp.inf)
        p = jax.nn.softmax(s, axis=-1)
        return jnp.einsum('bhqk,bqkhd->bqhd', p, vg.astype(f32))

    qpos = jnp.arange(L).reshape(nb, DSA_QBLOCK)
    out = lax.map(one_block, (blocks(qh), blocks(iqh), blocks(iwh), qpos))
    out = jnp.moveaxis(out, 0, 1).reshape(B, L, DSA_WIDTH)
    return rms_norm(out, norm_g).astype(q.dtype)


def s5_mixer(u, a_re, a_im, log_step, b_re, b_im, c_re, c_im, d, glu_w, glu_b, norm_g):
    B, L, _ = u.shape
    f32 = jnp.float32
    ug = u.astype(f32).reshape(B, L, S5_GROUPS, S5_GROUP_CH)
    dt = jnp.exp(log_step.astype(f32))[:, None]
    ar, ai = a_re.astype(f32), a_im.astype(f32)
    mag = jnp.exp(ar * dt)
    lam_re, lam_im = mag * jnp.cos(ai * dt), mag * jnp.sin(ai * dt)
    den = ar * ar + ai * ai
    nr, ni = lam_re - 1.0, lam_im
    coef_re = (nr * ar + ni * ai) / den
    coef_im = (ni * ar - nr * ai) / den
    br, bi = b_re.astype(f32), b_im.astype(f32)
    bb_re = coef_re[..., None] * br - coef_im[..., None] * bi
    bb_im = coef_re[..., None] * bi + coef_im[..., None] * br
    x_re = jnp.einsum('blgc,gpc->blgp', ug, bb_re)
    x_im = jnp.einsum('blgc,gpc->blgp', ug, bb_im)
    a_re_t = jnp.broadcast_to(lam_re, (1, L) + lam_re.shape)
    a_im_t = jnp.broadcast_to(lam_im, (1, L) + lam_im.shape)

    def combine(e1, e2):
        a1r, a1i, x1r, x1i = e1
        a2r, a2i, x2r, x2i = e2
        return (a2r * a1r - a2i * a1i, a2r * a1i + a2i * a1r,
                a2r * x1r - a2i * x1i + x2r, a2r * x1i + a2i * x1r + x2i)

    _, _, h_re, h_im = lax.associative_scan(combine, (a_re_t, a_im_t, x_re, x_im), axis=1)
    y = (jnp.einsum('blgp,gcp->blgc', h_re, c_re.astype(f32))
         - jnp.einsum('blgp,gcp->blgc', h_im, c_im.astype(f32)))
    y = (y + d.astype(f32) * ug).reshape(B, L, S5_WIDTH)
    y = jax.nn.gelu(y)
    y = y * jax.nn.sigmoid(y @ glu_w.astype(f32) + glu_b.astype(f32))
    return rms_norm(y, norm_g).astype(u.dtype)


def token_mix(h, w_in, gla_gate_w, gla_gate_b, gla_norm_g, dsa_norm_g, s5_a_re, s5_a_im,
              s5_log_step, s5_b_re, s5_b_im, s5_c_re, s5_c_im, s5_d, s5_glu_w, s5_glu_b,
              s5_norm_g, w_out):
    proj = h @ w_in
    offsets = np.cumsum(IN_SIZES)[:-1].tolist()
    (g_q, g_k, g_v, g_lr, g_r, d_q, d_k, d_v, i_q, i_k, i_w, s_u) = jnp.split(proj, offsets, axis=-1)
    o_gla = gla_mixer(g_q, g_k, g_v, g_lr, g_r, gla_gate_w, gla_gate_b, gla_norm_g)
    o_dsa = dsa_mixer(d_q, d_k, d_v, i_q, i_k, i_w, dsa_norm_g)
    o_s5 = s5_mixer(s_u, s5_a_re, s5_a_im, s5_log_step, s5_b_re, s5_b_im, s5_c_re, s5_c_im,
                    s5_d, s5_glu_w, s5_glu_b, s5_norm_g)
    return jnp.concatenate([o_gla, o_dsa, o_s5], axis=-1) @ w_out


def hier_moe(h, wg, bg, we, be, w_gate, w_up, w_down):
    B, L, D = h.shape
    T = B * L
    f32 = jnp.float32
    xt = h.reshape(T, D)
    grp_prob = jax.nn.softmax((xt @ wg + bg).astype(f32), axis=-1)
    grp_p, grp_i = lax.top_k(grp_prob, 1)
    exp_logits = (xt @ we + be).astype(f32).reshape(T, MOE_GROUPS, MOE_EXPERTS_PER_GROUP)
    in_grp = jnp.take_along_axis(exp_logits, grp_i[:, :, None], axis=1)[:, 0]
    top_l, top_j = lax.top_k(in_grp, MOE_TOPK)
    weights = grp_p * jax.nn.softmax(top_l, axis=-1)
    expert = grp_i * MOE_EXPERTS_PER_GROUP + top_j
    N = T * MOE_TOPK
    flat_e = expert.reshape(N)
    flat_tok = jnp.repeat(jnp.arange(T), MOE_TOPK)
    flat_w = weights.reshape(N)
    order = jnp.argsort(flat_e)
    se = flat_e[order]
    tok_sorted = flat_tok[order]
    counts = jnp.bincount(flat_e, length=MOE_EXPERTS)
    padded = (counts + MOE_BLOCK - 1) // MOE_BLOCK * MOE_BLOCK
    start = jnp.cumsum(counts) - counts
    pend = jnp.cumsum(padded)
    pstart = pend - padded
    dest = pstart[se] + jnp.arange(N) - start[se]
    n_blocks = -(-N // MOE_BLOCK) + MOE_EXPERTS
    rows = jnp.zeros((n_blocks * MOE_BLOCK, D), h.dtype).at[dest].set(xt[tok_sorted])
    block_e = jnp.minimum(jnp.searchsorted(pend, jnp.arange(n_blocks) * MOE_BLOCK, side='right'),
                          MOE_EXPERTS - 1)

    def run(inp):
        xb, e = inp
        hid = jax.nn.silu(xb @ w_gate[e]) * (xb @ w_up[e])
        return hid @ w_down[e]

    out = lax.map(run, (rows.reshape(n_blocks, MOE_BLOCK, D), block_e)).reshape(-1, D)
    contrib = out[dest] * flat_w[order][:, None].astype(out.dtype)
    y = jnp.zeros((T, D), out.dtype).at[tok_sorted].add(contrib)
    return y.reshape(B, L, D)


def setup_inputs(seed: int = 0) -> dict:
    key = jax.random.key(seed)
    ks = iter(jax.random.split(key, 40))

    def nrm(shape, s):
        return jax.random.normal(next(ks), shape, jnp.float32) * s

    D = D_MODEL
    n = jnp.arange(S5_STATE, dtype=jnp.float32)
    return {
        'x': nrm((BATCH, SEQ, D), 1.0),
        'c': nrm((BATCH, D), 1.0),
        'ada_w': nrm((DEPTH, D, 6 * D), 0.5 * D ** -0.5),
        'ada_b': nrm((DEPTH, 6 * D), 0.02),
        'norm1_g': 1.0 + nrm((DEPTH, D), 0.02),
        'w_in': nrm((DEPTH, D, D_IN), D ** -0.5),
        'gla_gate_w': nrm((DEPTH, GLA_RANK, GLA_QK), GLA_RANK ** -0.5),
        'gla_gate_b': nrm((DEPTH, GLA_QK), 0.1),
        'gla_norm_g': 1.0 + nrm((DEPTH, GLA_DV), 0.02),
        'dsa_norm_g': 1.0 + nrm((DEPTH, DSA_WIDTH), 0.02),
        's5_a_re': -0.5 * (1.0 + nrm((DEPTH, S5_GROUPS, S5_STATE), 0.01)),
        's5_a_im': jnp.broadcast_to(math.pi * n, (DEPTH, S5_GROUPS, S5_STATE)),
        's5_log_step': jax.random.uniform(next(ks), (DEPTH, S5_GROUPS), jnp.float32,
                                          minval=math.log(1e-3), maxval=math.log(1e-1)),
        's5_b_re': nrm((DEPTH, S5_GROUPS, S5_STATE, S5_GROUP_CH), (2 * S5_GROUP_CH) ** -0.5),
        's5_b_im': nrm((DEPTH, S5_GROUPS, S5_STATE, S5_GROUP_CH), (2 * S5_GROUP_CH) ** -0.5),
        's5_c_re': nrm((DEPTH, S5_GROUPS, S5_GROUP_CH, S5_STATE), (2 * S5_STATE) ** -0.5),
        's5_c_im': nrm((DEPTH, S5_GROUPS, S5_GROUP_CH, S5_STATE), (2 * S5_STATE) ** -0.5),
        's5_d': nrm((DEPTH, S5_GROUPS, S5_GROUP_CH), 0.5),
        's5_glu_w': nrm((DEPTH, S5_WIDTH, S5_WIDTH), S5_WIDTH ** -0.5),
        's5_glu_b': nrm((DEPTH, S5_WIDTH), 0.02),
        's5_norm_g': 1.0 + nrm((DEPTH, S5_WIDTH), 0.02),
        'w_out': nrm((DEPTH, D_MIX, D), D_MIX ** -0.5),
        'norm2_g': 1.0 + nrm((DEPTH, D), 0.02),
        'router_grp_w': nrm((DEPTH, D, MOE_GROUPS), D ** -0.5),
        'router_grp_b': nrm((DEPTH, MOE_GROUPS), 0.01),
        'router_exp_w': nrm((DEPTH, D, MOE_EXPERTS), D ** -0.5),
        'router_exp_b': nrm((DEPTH, MOE_EXPERTS), 0.01),
        'exp_w_gate': nrm((DEPTH, MOE_EXPERTS, D, MOE_HIDDEN), D ** -0.5),
        'exp_w_up': nrm((DEPTH, MOE_EXPERTS, D, MOE_HIDDEN), D ** -0.5),
        'exp_w_down': nrm((DEPTH, MOE_EXPERTS, MOE_HIDDEN, D), MOE_HIDDEN ** -0.5),
        'final_norm_g': 1.0 + nrm((D,), 0.02),
    }


def reference(x, c, ada_w, ada_b, norm1_g, w_in, gla_gate_w, gla_gate_b, gla_norm_g, dsa_norm_g,
              s5_a_re, s5_a_im, s5_log_step, s5_b_re, s5_b_im, s5_c_re, s5_c_im, s5_d, s5_glu_w,
              s5_glu_b, s5_norm_g, w_out, norm2_g, router_grp_w, router_grp_b, router_exp_w,
              router_exp_b, exp_w_gate, exp_w_up, exp_w_down, final_norm_g):
    cond = jax.nn.silu(c)
    for l in range(DEPTH):
        mod = (cond @ ada_w[l] + ada_b[l])[:, None, :]
        sh1, sc1, gt1, sh2, sc2, gt2 = jnp.split(mod, 6, axis=-1)
        h = rms_norm(x, norm1_g[l]) * (1.0 + sc1) + sh1
        x = x + gt1 * token_mix(h, w_in[l], gla_gate_w[l], gla_gate_b[l], gla_norm_g[l],
                                dsa_norm_g[l], s5_a_re[l], s5_a_im[l], s5_log_step[l],
                                s5_b_re[l], s5_b_im[l], s5_c_re[l], s5_c_im[l], s5_d[l],
                                s5_glu_w[l], s5_glu_b[l], s5_norm_g[l], w_out[l])
        h = rms_norm(x, norm2_g[l]) * (1.0 + sc2) + sh2
        x = x + gt2 * hier_moe(h, router_grp_w[l], router_grp_b[l], router_exp_w[l],
                               router_exp_b[l], exp_w_gate[l], exp_w_up[l], exp_w_down[l])
    return rms_norm(x, final_norm_g)
```

```python
import os
import numpy as np
import concourse.bass as bass
import concourse.mybir as mybir

F32 = mybir.dt.float32
BF16 = mybir.dt.bfloat16
I32 = mybir.dt.int32
U32 = mybir.dt.uint32
ALU = mybir.AluOpType
AF = mybir.ActivationFunctionType
AX = mybir.AxisListType

SEM_LIMIT = 30000


class KB:
    def __init__(self, nc, n_dma_sems=32, n_eng_sems=6):
        self.nc = nc
        self.E = {"pe": nc.tensor, "dve": nc.vector, "act": nc.scalar, "pool": nc.gpsimd, "sp": nc.sync}
        self.eng_sems = {}
        self.eng_cur = {}
        for e in ("pe", "dve", "act", "pool"):
            self.eng_sems[e] = [nc.alloc_semaphore(f"s_{e}{i}") for i in range(n_eng_sems)]
            self.eng_cur[e] = [0, 0]
        self.dma_pools = {
            "hw": [[nc.alloc_semaphore(f"s_dma{i}"), 0, None] for i in range(n_dma_sems)],
            "sw": [[nc.alloc_semaphore(f"s_swdma{i}"), 0, None] for i in range(40)],
        }
        self.dma_rr = {"hw": 0, "sw": 0}
        self.seen = {e: {} for e in self.E}
        self.last_w = {}
        self.readers = {}
        self.psum_keys = set()
        self.n_ins = 0
        self.n_wait = 0
        self._uid = 0

    def sb(self, name, shape, dt=F32):
        return self.nc.alloc_sbuf_tensor(name, list(shape), dt)

    def barrier(self):
        toks = []
        for e in ("pe", "dve", "act", "pool"):
            cur = self.eng_cur[e]
            if cur[1] > 0:
                toks.append((self.eng_sems[e][cur[0]], cur[1]))
        for pool in self.dma_pools.values():
            for slot in pool:
                if slot[1] > 0:
                    toks.append((slot[0], slot[1]))
        for eng in self.E:
            for sem, val in toks:
                self._wait(eng, sem, val)

    def scope(self, prefix):
        kb = self
        import contextlib

        class _Scope:
            def __enter__(s):
                s.es = contextlib.ExitStack()
                s.es.__enter__()
                return s

            def __call__(s, name, shape, dt=F32):
                kb._uid += 1
                return s.es.enter_context(kb.nc.sbuf_tensor(f"{prefix}_{name}_{kb._uid}", list(shape), dt))

            def __exit__(s, *a):
                if a[0] is None:
                    kb.barrier()
                return s.es.__exit__(*a)
        return _Scope()

    def ps(self, name, shape, dt=F32):
        self.psum_keys.add(name)
        return self.nc.alloc_psum_tensor(name, list(shape), dt)

    @staticmethod
    def key(a):
        if isinstance(a, (str, tuple)):
            return a
        t = getattr(a, "tensor", None)
        return t.name if t is not None else a.name

    def _wait(self, eng, sem, val):
        sid = id(sem)
        d = self.seen[eng]
        if d.get(sid, 0) >= val:
            return
        self.E[eng].wait_ge(sem, val)
        d[sid] = val
        self.n_wait += 1

    def _deps(self, eng, reads, writes):
        deps = {}
        def add(tok):
            if tok is None:
                return
            sem, val, teng = tok
            if teng == "pe" and eng == "pe":
                return
            k = id(sem)
            if k not in deps or deps[k][1] < val:
                deps[k] = (sem, val)
        for r in reads:
            add(self.last_w.get(r))
        for w in writes:
            add(self.last_w.get(w))
            for t in self.readers.get(w, ()):
                add(t)
        for sem, val in deps.values():
            self._wait(eng, sem, val)

    def _commit(self, tok, reads, writes):
        for r in reads:
            self.readers.setdefault(r, []).append(tok)
        for w in writes:
            self.last_w[w] = tok
            self.readers[w] = []

    def op(self, eng, fn, reads, writes):
        reads = [self.key(r) for r in reads if r is not None]
        writes = [self.key(w) for w in writes if w is not None]
        writes = writes + [r for r in reads if r in self.psum_keys and r not in writes]
        self._deps(eng, reads, writes)
        ins = fn(self.E[eng])
        cur = self.eng_cur[eng]
        if cur[1] >= SEM_LIMIT:
            cur[0] += 1
            cur[1] = 0
        cur[1] += 1
        sem = self.eng_sems[eng][cur[0]]
        ins.then_inc(sem, 1)
        self.n_ins += 1
        self._commit((sem, cur[1], eng), reads, writes)
        return ins

    def _dma_slot(self, eng):
        kind = "sw" if eng == "pool" else "hw"
        pool = self.dma_pools[kind]
        slot = pool[self.dma_rr[kind]]
        self.dma_rr[kind] = (self.dma_rr[kind] + 1) % len(pool)
        if slot[1] >= SEM_LIMIT:
            raise RuntimeError("dma sem overflow")
        return slot

    def dma(self, eng, out, in_, reads=None, writes=None, sync=False, **kw):
        if kw.get("allow_slow_non_contiguous"):
            sync = True
        reads = [self.key(r) for r in (reads if reads is not None else [in_])]
        writes = [self.key(w) for w in (writes if writes is not None else [out])]
        slot = self._dma_slot(eng)
        if slot[1] > 0:
            self._wait(eng, slot[0], slot[1])
        self._deps(eng, reads, writes)
        ins = self.E[eng].dma_start(out=out, in_=in_, **kw)
        slot[1] += 16
        ins.then_inc(slot[0], 16)
        self.n_ins += 1
        self._commit((slot[0], slot[1], "dma"), reads, writes)
        if sync:
            self._wait(eng, slot[0], slot[1])
        return ins

    def idma(self, out, out_off, in_, in_off, reads, writes, **kw):
        eng = "pool"
        reads = [self.key(r) for r in reads]
        writes = [self.key(w) for w in writes]
        slot = self._dma_slot(eng)
        if slot[1] > 0:
            self._wait(eng, slot[0], slot[1])
        self._deps(eng, reads, writes)
        ins = self.nc.gpsimd.indirect_dma_start(out=out, out_offset=out_off, in_=in_, in_offset=in_off, **kw)
        slot[1] += 16
        ins.then_inc(slot[0], 16)
        self.n_ins += 1
        self._commit((slot[0], slot[1], "dma"), reads, writes)
        return ins

    def finish(self, keys):
        for k in keys:
            tok = self.last_w.get(self.key(k))
            if tok is not None:
                self._wait("sp", tok[0], tok[1])

    def mm(self, out, lhsT, rhs, start=True, stop=True, reads=None, writes=None):
        r = reads if reads is not None else [lhsT, rhs]
        w = writes if writes is not None else [out]
        return self.op("pe", lambda e: e.matmul(out, lhsT, rhs, start=start, stop=stop), r, w)

    def tr(self, out, in_, ident, reads=None, writes=None):
        r = reads if reads is not None else [in_, ident]
        w = writes if writes is not None else [out]
        return self.op("pe", lambda e: e.transpose(out, in_, ident), r, w)

    def act(self, out, in_, func, bias=None, scale=None, accum_out=None, eng="act", reads=None, writes=None):
        kw = {}
        r = [in_]
        if bias is not None:
            kw["bias"] = bias
            if not isinstance(bias, (int, float)):
                r.append(bias)
        if scale is not None:
            kw["scale"] = scale
            if not isinstance(scale, (int, float)):
                r.append(scale)
        w = [out]
        if accum_out is not None:
            kw["accum_out"] = accum_out
            w.append(accum_out)
        if reads is not None:
            r = reads
        if writes is not None:
            w = writes
        return self.op("act", lambda e: e.activation(out, in_, func, **kw), r, w)

    def ts(self, eng, out, in0, s1, s2=None, op0=ALU.mult, op1=None, accum_out=None, reads=None, writes=None):
        r = [in0]
        if not isinstance(s1, (int, float)):
            r.append(s1)
        if s2 is not None and not isinstance(s2, (int, float)):
            r.append(s2)
        w = [out]
        kw = {}
        if op1 is not None:
            kw["op1"] = op1
        if accum_out is not None:
            kw["accum_out"] = accum_out
            w.append(accum_out)
        if reads is not None:
            r = reads
        if writes is not None:
            w = writes
        return self.op(eng, lambda e: e.tensor_scalar(out, in0, s1, s2, op0, **kw), r, w)

    def tt(self, eng, out, in0, in1, op, reads=None, writes=None):
        r = reads if reads is not None else [in0, in1]
        w = writes if writes is not None else [out]
        return self.op(eng, lambda e: e.tensor_tensor(out, in0, in1, op), r, w)

    def stt(self, eng, out, in0, scalar, in1, op0, op1, accum_out=None, reads=None, writes=None):
        r = [in0, in1]
        if not isinstance(scalar, (int, float)):
            r.append(scalar)
        w = [out]
        kw = {}
        if accum_out is not None:
            kw["accum_out"] = accum_out
            w.append(accum_out)
        if reads is not None:
            r = reads
        if writes is not None:
            w = writes
        return self.op(eng, lambda e: e.scalar_tensor_tensor(out, in0, scalar, in1, op0, op1, **kw), r, w)

    def copy(self, eng, out, in_, reads=None, writes=None):
        r = reads if reads is not None else [in_]
        w = writes if writes is not None else [out]
        if eng == "act":
            return self.op(eng, lambda e: e.copy(out, in_), r, w)
        return self.op(eng, lambda e: e.tensor_copy(out, in_), r, w)

    def memset(self, eng, out, val, writes=None):
        w = writes if writes is not None else [out]
        return self.op(eng, lambda e: e.memset(out, val), [], w)

    def reduce(self, eng, out, in_, op, axis=AX.X, reads=None, writes=None):
        r = reads if reads is not None else [in_]
        w = writes if writes is not None else [out]
        return self.op(eng, lambda e: e.tensor_reduce(out, in_, axis, op), r, w)

    def scan(self, eng, out, d0, d1, init, op0, op1, reads=None, writes=None):
        r = [d0, d1]
        if not isinstance(init, (int, float)):
            r.append(init)
        if reads is not None:
            r = reads
        w = writes if writes is not None else [out]
        return self.op(eng, lambda e: e.tensor_tensor_scan(out, d0, d1, init, op0, op1), r, w)

    def recip(self, eng, out, in_):
        return self.op(eng, lambda e: e.reciprocal(out, in_), [in_], [out])

    def rstd(self, ss, n, eps):
        self.ts("dve", ss, ss, 1.0 / n, eps, ALU.mult, ALU.add)
        self.act(ss, ss, AF.Sqrt)
        self.recip("dve", ss, ss)


L = int(os.environ.get('MK_L', 8192))
D = 1024
DIN = 2616
NT = L // 128
EPS = 1e-6
GSTOP = int(os.environ.get('GSTOP', 99))

C_GQ, C_GK, C_GV, C_GLR, C_GR = 0, 192, 384, 768, 784
C_DQ, C_DK, C_DV, C_IQ, C_IK, C_IW, C_SU = 1168, 1424, 1680, 1936, 2192, 2224, 2232

FM_PIECES = [(0, 128), (128, 256), (256, 384), (768, 784),
             (1168, 1296), (1296, 1424), (1424, 1552), (1552, 1680),
             (1936, 2064), (2064, 2192), (2192, 2224),
             (2232, 2360), (2360, 2488), (2488, 2616)]
TM_PIECES = [(384, 768), (784, 1168), (1680, 1936), (2224, 2232)]


class Ctx:
    pass


def tk(name, t0, t1):
    return [(name, i) for i in range(t0 // 128, (t1 + 127) // 128)]


def setup(nc, ext_scratch=()):
    c = Ctx()
    c.nc = nc
    k = KB(nc)
    c.k = k

    def inp(name, shape):
        return nc.dram_tensor(name, list(shape), F32, kind="ExternalInput").ap()

    def scr(name, shape, dt=F32):
        kind = "ExternalOutput" if name in ext_scratch else "Internal"
        return nc.dram_tensor(name, list(shape), dt, kind=kind).ap()

    c.inp, c.scr = inp, scr
    c.x = inp("x", [L, D])
    c.c = inp("c", [D])
    c.ada_w = inp("ada_w", [2, D, 6 * D])
    c.ada_b = inp("ada_b", [2, 6 * D])
    c.norm1_g = inp("norm1_g", [2, D])
    c.norm2_g = inp("norm2_g", [2, D])
    c.w_in = inp("w_in", [2, D, DIN])
    c.modrow = scr("modrow", [2, 6 * D])
    c.proj = scr("proj", [L, DIN])
    c.projT = scr("projT", [DIN, L])
    ident = np.eye(128, dtype=np.float32)
    c.ident_d = nc.inline_tensor(ident, "ident_c").ap()
    c.identf = k.sb("identf", [128, 128], F32)
    c.identb = k.sb("identb", [128, 128], BF16)
    k.dma("sp", c.identf[:], c.ident_d)
    k.copy("dve", c.identb[:], c.identf[:])
    c.P = [k.ps(f"bank{i}", [128, 512], F32) for i in range(8)]
    return c


def adaln(c, l):
    k, nc = c.k, c.nc
    with k.scope(f"ada{l}") as al:
        _adaln(c, l, al)


def modchunk(c, al, l, idx, name):
    t = al(name, [128, D])
    c.k.dma("sp", t[:], c.modrow[l:l + 1, idx * D:(idx + 1) * D].partition_broadcast(128))
    return t


def _adaln(c, l, al):
    k, nc = c.k, c.nc
    c.condT = al("condT", [128, 8], F32)
    c.adaw = [al(f"adaw{i}", [128, 8, 512], F32) for i in range(2)]
    c.modsb = al("modsb", [1, 6 * D], F32)
    c.adab = al("adab", [1, 6 * D], F32)
    c.gtmp = al("gtmp", [1, D], F32)
    k.dma("sp", c.condT[:], c.c.rearrange("(kc p) -> p kc", p=128), allow_slow_non_contiguous=True)
    k.act(c.condT[:], c.condT[:], AF.Silu)
    k.dma("sp", c.adab[:], c.ada_b[l:l + 1, :])
    for cc in range(12):
        wt = c.adaw[cc % 2]
        k.dma("sp" if cc % 2 == 0 else "pool", wt[:],
              c.ada_w[l, :, cc * 512:(cc + 1) * 512].rearrange("(kc p) n -> p kc n", p=128))
        pb = c.P[cc % 2]
        for kc in range(8):
            k.mm(pb[0:1, :], c.condT[:, kc:kc + 1], wt[:, kc, :], start=(kc == 0), stop=(kc == 7))
        k.tt("dve", c.modsb[0:1, cc * 512:(cc + 1) * 512], pb[0:1, :], c.adab[0:1, cc * 512:(cc + 1) * 512], ALU.add)
    for (gname, sc_chunk) in (("norm1_g", 1), ("norm2_g", 4)):
        g_ap = getattr(c, gname)
        k.dma("sp", c.gtmp[:], g_ap[l:l + 1, :])
        sl = c.modsb[0:1, sc_chunk * D:(sc_chunk + 1) * D]
        k.stt("dve", sl, sl, 1.0, c.gtmp[:], ALU.add, ALU.mult)
    k.dma("sp", c.modrow[l:l + 1, :], c.modsb[:])


def load_w_bf16(c, al, dst, src_ap, ncols, nkc=8):
    k = c.k
    tmps = [al(f"wtmp{i}", [128, ncols], F32) for i in range(2)]
    for kc in range(nkc):
        t = tmps[kc % 2]
        k.dma("sp" if kc % 2 == 0 else "pool", t[:, :ncols], src_ap[kc * 128:(kc + 1) * 128, :])
        k.copy("dve" if kc % 2 == 0 else "pool", dst[:, kc, :], t[:, :ncols])


def phase_a(c, l, xsrc):
    with c.k.scope(f"pa{l}") as al:
        _phase_a(c, l, xsrc, al)


def _phase_a(c, l, xsrc, al):
    k, nc = c.k, c.nc
    c.wbf = al("wbf", [128, 8, DIN], BF16)
    c.xt = [al(f"xt{i}", [128, D], F32) for i in range(2)]
    c.hn = al("hn", [128, D], F32)
    c.hb = [al(f"hb{i}", [128, D], BF16) for i in range(2)]
    c.hT = [al(f"hT{i}", [128, 8, 512], BF16) for i in range(2)]
    c.ss = al("ss", [128, 4], F32)
    c.junk = al("junk", [128, D], F32)
    c.stm = [al(f"stm{i}", [128, 1032], F32) for i in range(2)]
    c.sfm = [al(f"sfm{i}", [128, 512], F32) for i in range(3)]
    with k.scope(f"pa{l}w") as al2:
        load_w_bf16(c, al2, c.wbf, c.w_in[l], DIN)
    A1 = modchunk(c, al, l, 1, "A1")[:]
    SH1 = modchunk(c, al, l, 0, "SH1")[:]
    tp = c.P[7][:].bitcast(BF16)
    nfm = 0
    for g in range(L // 512):
        hT = c.hT[g % 2]
        for t in range(4):
            ti = g * 4 + t
            xt = c.xt[ti % 2]
            hb = c.hb[ti % 2]
            k.dma("act", xt[:], xsrc[ti * 128:(ti + 1) * 128, :])
            ssc = c.ss[:, t:t + 1]
            k.act(c.junk[:], xt[:], AF.Square, accum_out=ssc)
            k.rstd(ssc, D, EPS)
            k.stt("dve", c.hn[:], xt[:], ssc, A1, ALU.mult, ALU.mult)
            k.tt("pool", hb[:], c.hn[:], SH1, ALU.add)
            for kc in range(8):
                k.tr(tp[:, kc * 128:(kc + 1) * 128], hb[:, kc * 128:(kc + 1) * 128], c.identb[:])
            k.copy("act", hT[:, :, t * 128:(t + 1) * 128], tp.rearrange("p (a b) -> p a b", a=8))
            stm = c.stm[ti % 2]
            off = 0
            for pi, (c0, c1) in enumerate(TM_PIECES):
                w = c1 - c0
                pb = c.P[pi % 2]
                for kc in range(8):
                    k.mm(pb[:, :w], hT[:, kc, t * 128:(t + 1) * 128], c.wbf[:, kc, c0:c1], start=(kc == 0), stop=(kc == 7))
                k.copy("act" if pi % 2 == 0 else "dve", stm[:, off:off + w], pb[:, :w])
                off += w
            off = 0
            for (c0, c1) in TM_PIECES:
                w = c1 - c0
                k.dma("sp", c.proj[ti * 128:(ti + 1) * 128, c0:c1], stm[:, off:off + w],
                      writes=tk("proj", ti * 128, ti * 128 + 128))
                off += w
        for pi, (c0, c1) in enumerate(FM_PIECES):
            m = c1 - c0
            pb = c.P[2 + pi % 3]
            sf = c.sfm[nfm % 3]
            nfm += 1
            for kc in range(8):
                k.mm(pb[:m, :], c.wbf[:, kc, c0:c1], hT[:, kc, :], start=(kc == 0), stop=(kc == 7))
            k.copy("act" if pi % 2 == 0 else "dve", sf[:m, :], pb[:m, :])
            k.dma("sp", c.projT[c0:c1, g * 512:(g + 1) * 512], sf[:m, :], writes=tk("projT", g * 512, g * 512 + 512))


def bc_last(ap2, n):
    return ap2.unsqueeze(2).to_broadcast([ap2.shape[0], ap2.shape[1], n])


def bc_mid(ap2, n):
    return ap2.unsqueeze(1).to_broadcast([ap2.shape[0], n, ap2.shape[1]])


def gla_setup(c):
    nc = c.nc
    c.gla_gate_w = c.inp("gla_gate_w", [2, 16, 192])
    c.gla_gate_b = c.inp("gla_gate_b", [2, 192])
    c.gla_norm_g = c.inp("gla_norm_g", [2, 96])
    s = np.arange(128)
    m = ((s[:, None] // 64) == (s[None, :] // 64)) & (s[:, None] <= s[None, :])
    c.tribd_d = nc.inline_tensor((m / 16.0).astype(np.float32), "tribd_c").ap()
    m64 = (s[:64, None] <= s[None, :64]).astype(np.float32)
    c.maskbd_d = nc.inline_tensor(np.tile(m64, (1, 8)), "maskbd_c").ap()
    c.mixcat = c.scr("mixcat", [L, D])


def phase_gla(c, l):
    with c.k.scope(f"gla{l}") as al:
        _phase_gla(c, l, al)


def _phase_gla(c, l, al):
    k, nc = c.k, c.nc
    P = c.P
    tribd = al("tribd", [128, 128])
    maskbd = al("maskbd", [64, 512])
    gw = al("gw", [17, 192])
    gnorm = al("gnorm", [64, 96])
    lrT = [al(f"lrT{i}", [17, 128]) for i in range(2)]
    g16_2 = [al(f"g16{i}", [128, 192]) for i in range(2)]
    ta_2 = [al(f"ta{i}", [128, 192]) for i in range(2)]
    tb_2 = [al(f"tb{i}", [128, 192]) for i in range(2)]
    Eq_2 = [al(f"Eq{i}", [48, 4, 128]) for i in range(2)]
    Ek_2 = [al(f"Ek{i}", [48, 4, 128]) for i in range(2)]
    qk = [al(f"qk{i}", [48, 8, 128]) for i in range(2)]
    qd_2 = [al(f"qd{i}", [48, 4, 128], BF16) for i in range(2)]
    kd_2 = [al(f"kd{i}", [48, 4, 128], BF16) for i in range(2)]
    k2T_2 = [al(f"k2T{i}", [48, 4, 128]) for i in range(2)]
    k2b_2 = [al(f"k2b{i}", [64, 8, 48], BF16) for i in range(2)]
    vr = [al(f"vr{i}", [64, 2, 768]) for i in range(2)]
    vb_2 = [al(f"vb{i}", [64, 2, 384], BF16) for i in range(2)]
    attb_2 = [al(f"attb{i}", [64, 8, 64], BF16) for i in range(2)]
    S = al("S", [48, 4, 96])
    Sb = [al(f"Sb{i}", [48, 4, 96], BF16) for i in range(2)]
    stmp = al("stmp", [48, 4, 96])
    osq = al("osq", [64, 4, 96])
    ss = al("ss", [64, 4])
    on = al("on", [64, 4, 96])
    sr = al("sr", [64, 384])
    oo = [al(f"oo{i}", [64, 2, 384]) for i in range(2)]

    k.dma("sp", tribd[:], c.tribd_d)
    k.dma("sp", maskbd[:], c.maskbd_d)
    k.dma("sp", gw[0:1, :], c.gla_gate_b[l:l + 1, :])
    k.dma("sp", gw[1:17, :], c.gla_gate_w[l])
    k.dma("sp", gnorm[:], c.gla_norm_g[l:l + 1, :].partition_broadcast(64))
    for i in range(2):
        k.memset("dve", lrT[i][0:1, :], 1.0)
    k.memset("dve", S[:], 0.0)
    k.memset("dve", Sb[0][:], 0.0)
    scur = 0
    maskv = maskbd[:].rearrange("p (a c) -> p a c", a=8)
    for ti in range(NT):
        t0 = ti * 128
        g16, ta, tb, Eq, Ek, k2T, qd, kd, k2b, vb, attb = (x_[ti % 2] for x_ in
            (g16_2, ta_2, tb_2, Eq_2, Ek_2, k2T_2, qd_2, kd_2, k2b_2, vb_2, attb_2))
        lt = lrT[ti % 2]
        k.dma("act", lt[1:17, :], c.projT[C_GLR:C_GLR + 16, t0:t0 + 128], reads=tk("projT", t0, t0 + 128))
        q_k = qk[ti % 2]
        k.dma("act", q_k[:], c.projT[0:384, t0:t0 + 128].rearrange("(h d) t -> d h t", d=48),
              reads=tk("projT", t0, t0 + 128))
        v_r = vr[ti % 2]
        k.dma("pool", v_r[:, :, 0:384], c.proj[t0:t0 + 128, C_GV:C_GV + 384].rearrange("(ch p) e -> p ch e", p=64),
              reads=tk("proj", t0, t0 + 128))
        k.dma("pool", v_r[:, :, 384:768], c.proj[t0:t0 + 128, C_GR:C_GR + 384].rearrange("(ch p) e -> p ch e", p=64),
              reads=tk("proj", t0, t0 + 128))
        k.mm(P[0][:, :192], lt[:], gw[:])
        z = P[0][:, :192]
        k.ts("dve", tb[:], z, 0.0, None, ALU.min)
        k.stt("dve", ta[:], tb[:], -2.0, z, ALU.mult, ALU.add)
        k.act(ta[:], ta[:], AF.Exp, scale=-1.0)
        k.act(ta[:], ta[:], AF.Ln, bias=1.0)
        k.tt("dve", g16[:], tb[:], ta[:], ALU.subtract)
        for h in range(4):
            k.mm(P[1][:48, h * 128:(h + 1) * 128], g16[:, h * 48:(h + 1) * 48], tribd[:])
        bcp = P[1][:48, :].rearrange("p (h c) -> p h c", h=4)
        k.act(Eq[:], bcp, AF.Exp)
        k.act(Ek[:], bcp, AF.Exp, scale=-1.0)
        k.stt("dve", qd[:], q_k[:, 0:4, :], float(48 ** -0.5), Eq[:], ALU.mult, ALU.mult)
        k.tt("pool", kd[:], q_k[:, 4:8, :], Ek[:], ALU.mult)
        for h in range(4):
            for ch in range(2):
                sl = slice(ch * 64, ch * 64 + 64)
                k.stt("dve", k2T[:, h, sl], q_k[:, 4 + h, sl], Eq[:, h, ch * 64 + 63:ch * 64 + 64], Ek[:, h, sl],
                      ALU.mult, ALU.mult)
        k.copy("act", vb[:], v_r[:, :, 0:384])
        for ch in range(2):
            sl = slice(ch * 64, ch * 64 + 64)
            for h in range(4):
                a = ch * 4 + h
                k.mm(P[2][:64, a * 64:(a + 1) * 64], kd[:, h, sl], qd[:, h, sl])
        k.tt("dve", attb[:], P[2][:64, :].rearrange("p (a c) -> p a c", a=8), maskv, ALU.mult)
        for ch in range(2):
            sl = slice(ch * 64, ch * 64 + 64)
            for h in range(4):
                a = ch * 4 + h
                k.tr(P[3][:64, a * 48:(a + 1) * 48], k2T[:, h, sl], c.identf[:48, :48])
        k.copy("act", k2b[:], P[3][:64, :384].rearrange("p (a d) -> p a d", a=8))
        o_ = oo[ti % 2]
        for ch in range(2):
            sl = slice(ch * 64, ch * 64 + 64)
            po = P[4 + ch]
            pov = po[:64, :384].rearrange("p (h e) -> p h e", h=4)
            Sin = Sb[scur]
            for h in range(4):
                a = ch * 4 + h
                k.mm(pov[:, h, :], attb[:, a, :], vb[:, ch, h * 96:(h + 1) * 96], start=True, stop=False)
                k.mm(pov[:, h, :], qd[:, h, sl], Sin[:, h, :], start=False, stop=True)
            puv = P[6][:48, :384].rearrange("p (h e) -> p h e", h=4)
            for h in range(4):
                a = ch * 4 + h
                k.mm(puv[:, h, :], k2b[:, a, :], vb[:, ch, h * 96:(h + 1) * 96])
            dec = Eq[:, :, ch * 64 + 63:ch * 64 + 64].rearrange("p h o -> p (h o)")
            k.tt("dve", stmp[:], S[:], bc_last(dec, 96), ALU.mult)
            k.tt("dve", S[:], stmp[:], puv, ALU.add)
            scur = 1 - scur
            k.copy("act", Sb[scur][:], S[:])
            k.act(osq[:], pov, AF.Square)
            k.reduce("dve", ss[:], osq[:], ALU.add)
            k.rstd(ss[:], 96, EPS)
            k.tt("dve", on[:], pov, bc_last(ss[:], 96), ALU.mult)
            k.tt("pool", on[:], on[:], bc_mid(gnorm[:], 4), ALU.mult)
            k.act(sr[:], v_r[:, ch, 384:768], AF.Silu)
            k.tt("pool", o_[:, ch, :], on[:].rearrange("p h e -> p (h e)"), sr[:], ALU.mult)
        k.dma("sp", c.mixcat[t0:t0 + 128, 0:384].rearrange("(ch p) e -> p ch e", p=64), o_[:],
              writes=tk("mixgla", t0, t0 + 128))


S5T = 512
TWO_PI = float(2 * np.pi)


def s5_setup(c):
    nc = c.nc
    for n, shp in (("s5_a_re", [2, 24, 64]), ("s5_a_im", [2, 24, 64]), ("s5_log_step", [2, 24]),
                   ("s5_b_re", [2, 24, 64, 16]), ("s5_b_im", [2, 24, 64, 16]),
                   ("s5_c_re", [2, 24, 16, 64]), ("s5_c_im", [2, 24, 16, 64]), ("s5_d", [2, 24, 16]),
                   ("s5_glu_w", [2, 384, 384]), ("s5_glu_b", [2, 384]), ("s5_norm_g", [2, 384])):
        setattr(c, n, c.inp(n, shp))
    c.jrow_d = nc.inline_tensor(np.arange(S5T, dtype=np.float32)[None, :], "jrow_c").ap()


def sincos(k, al, S, C, A, shape, tag):
    q = al(f"sc_q{tag}", shape)
    ph = al(f"sc_p{tag}", shape)
    for (out, shift) in ((S, 0.0), (C, float(np.pi / 2))):
        k.ts("dve", q[:], A, shift, 1.0 / TWO_PI, ALU.add, ALU.mult)
        k.ts("dve", q[:], q[:], 12582912.0, None, ALU.add)
        k.ts("dve", q[:], q[:], 12582912.0, None, ALU.subtract)
        k.stt("dve", ph[:], q[:], -TWO_PI, A, ALU.mult, ALU.add)
        k.ts("dve", ph[:], ph[:], shift, 3.1415925, ALU.add, ALU.min)
        k.ts("dve", ph[:], ph[:], -3.1415925, None, ALU.max)
        k.act(out, ph[:], AF.Sin)


def phase_s5(c, l):
    with c.k.scope(f"s5{l}") as al:
        _phase_s5(c, l, al)


def _phase_s5(c, l, al):
    k, nc = c.k, c.nc
    P = c.P
    T = S5T
    NJ = 12
    ar = al("ar", [128, NJ]); ai = al("ai", [128, NJ]); ls = al("ls", [128, NJ])
    k.dma("sp", ar[:], c.s5_a_re[l].rearrange("(j two) p -> (two p) j", two=2), allow_slow_non_contiguous=True)
    k.dma("sp", ai[:], c.s5_a_im[l].rearrange("(j two) p -> (two p) j", two=2), allow_slow_non_contiguous=True)
    lsv = c.s5_log_step[l:l + 1, :].rearrange("o (j two) -> o two j", two=2)
    k.dma("sp", ls[0:64, :], lsv[:, 0, :].partition_broadcast(64), allow_slow_non_contiguous=True)
    k.dma("sp", ls[64:128, :], lsv[:, 1, :].partition_broadcast(64), allow_slow_non_contiguous=True)
    dt = al("dt", [128, NJ]); mag = al("mag", [128, NJ]); th = al("th", [128, NJ])
    cth = al("cth", [128, NJ]); sth = al("sth", [128, NJ]); nsth = al("nsth", [128, NJ])
    k.act(dt[:], ls[:], AF.Exp)
    k.tt("dve", mag[:], ar[:], dt[:], ALU.mult)
    k.act(mag[:], mag[:], AF.Exp)
    k.tt("dve", th[:], ai[:], dt[:], ALU.mult)
    q = al("q0", [128, NJ])
    k.ts("dve", q[:], th[:], 1.0 / TWO_PI, 12582912.0, ALU.mult, ALU.add)
    k.ts("dve", q[:], q[:], 12582912.0, None, ALU.subtract)
    k.stt("dve", th[:], q[:], -TWO_PI, th[:], ALU.mult, ALU.add)
    sincos(k, al, sth[:], cth[:], th[:], [128, NJ], "a")
    k.ts("dve", nsth[:], sth[:], -1.0, None, ALU.mult)
    lre = al("lre", [128, NJ]); lim = al("lim", [128, NJ]); den = al("den", [128, NJ])
    t1 = al("pt1", [128, NJ]); t2 = al("pt2", [128, NJ]); cre = al("cre", [128, NJ]); cim = al("cim", [128, NJ])
    k.tt("dve", lre[:], mag[:], cth[:], ALU.mult)
    k.tt("dve", lim[:], mag[:], sth[:], ALU.mult)
    k.tt("dve", den[:], ar[:], ar[:], ALU.mult)
    k.tt("dve", t1[:], ai[:], ai[:], ALU.mult)
    k.tt("dve", den[:], den[:], t1[:], ALU.add)
    k.recip("dve", den[:], den[:])
    k.ts("dve", lre[:], lre[:], -1.0, None, ALU.add)
    k.tt("dve", t1[:], lre[:], ar[:], ALU.mult)
    k.tt("dve", t2[:], lim[:], ai[:], ALU.mult)
    k.tt("dve", t1[:], t1[:], t2[:], ALU.add)
    k.tt("dve", cre[:], t1[:], den[:], ALU.mult)
    k.tt("dve", t1[:], lim[:], ar[:], ALU.mult)
    k.tt("dve", t2[:], lre[:], ai[:], ALU.mult)
    k.tt("dve", t1[:], t1[:], t2[:], ALU.subtract)
    k.tt("dve", cim[:], t1[:], den[:], ALU.mult)
    br = al("br", [128, NJ, 16]); bi = al("bi", [128, NJ, 16])
    k.dma("sp", br[:], c.s5_b_re[l].rearrange("(j two) p c -> (two p) j c", two=2), sync=True)
    k.dma("sp", bi[:], c.s5_b_im[l].rearrange("(j two) p c -> (two p) j c", two=2), sync=True)
    bbr = al("bbr", [128, NJ, 16]); bbi = al("bbi", [128, NJ, 16]); bt = al("bt", [128, NJ, 16])
    k.tt("dve", bbr[:], br[:], bc_last(cre[:], 16), ALU.mult)
    k.tt("dve", bt[:], bi[:], bc_last(cim[:], 16), ALU.mult)
    k.tt("dve", bbr[:], bbr[:], bt[:], ALU.subtract)
    k.tt("dve", bbi[:], bi[:], bc_last(cre[:], 16), ALU.mult)
    k.tt("dve", bt[:], br[:], bc_last(cim[:], 16), ALU.mult)
    k.tt("dve", bbi[:], bbi[:], bt[:], ALU.add)
    Mre = al("Mre", [128, NJ, 32]); Mim = al("Mim", [128, NJ, 32])
    BBT = [al("BBTre", [32, NJ, 128]), al("BBTim", [32, NJ, 128])]
    for (M, bb, dst) in ((Mre, bbr, BBT[0]), (Mim, bbi, BBT[1])):
        k.memset("dve", M[:], 0.0)
        k.copy("dve", M[0:64, :, 0:16], bb[0:64, :, :])
        k.copy("dve", M[64:128, :, 16:32], bb[64:128, :, :])
        for j4 in range(3):
            for jj in range(4):
                j = j4 * 4 + jj
                k.tr(P[0][:32, jj * 128:(jj + 1) * 128], M[:, j, :], c.identf[:])
            k.copy("act", dst[:, j4 * 4:(j4 + 1) * 4, :], P[0][:32, :].rearrange("p (a b) -> p a b", a=4))
    crt = al("crt", [128, NJ, 16]); cit = al("cit", [128, NJ, 16])
    cld = al("cld", [128, 128])
    for (src, dst) in ((c.s5_c_re, crt), (c.s5_c_im, cit)):
        for (j0, nj) in ((0, 8), (8, 4)):
            rows = nj * 16
            for jl in range(nj):
                for two in range(2):
                    k.dma("sp", cld[jl * 16:(jl + 1) * 16, two * 64:(two + 1) * 64], src[l, 2 * (j0 + jl) + two])
            k.tr(P[0][:, :rows], cld[:rows, :], c.identf[:rows, :rows])
            k.copy("act", dst[:, j0:j0 + nj, :], P[0][:, :rows].rearrange("p (j c) -> p j c", c=16))
    CR = al("CRpad", [128, NJ, 128]); CI = al("CIpad", [128, NJ, 128])
    k.memset("dve", CR[:], 0.0)
    k.memset("pool", CI[:], 0.0)
    for j in range(NJ):
        o0 = 32 * (j % 4)
        k.copy("dve", CR[0:64, j, o0:o0 + 16], crt[0:64, j, :])
        k.copy("dve", CR[64:128, j, o0 + 16:o0 + 32], crt[64:128, j, :])
        k.ts("dve", CI[0:64, j, o0:o0 + 16], cit[0:64, j, :], -1.0, None, ALU.mult)
        k.ts("dve", CI[64:128, j, o0 + 16:o0 + 32], cit[64:128, j, :], -1.0, None, ALU.mult)
    dcol = al("dcol", [128, 3])
    k.dma("sp", dcol[:], c.s5_d[l].rearrange("(jj g8) c -> (g8 c) jj", g8=8), allow_slow_non_contiguous=True)
    jrow = al("jrow", [128, T])
    k.dma("sp", jrow[:], c.jrow_d.partition_broadcast(128))
    Ct = al("Ct", [128, NJ, T]); St = al("St", [128, NJ, T])
    with k.scope(f"s5sc{l}") as al2:
        ang = al2("ang", [128, NJ, T])
        for j in range(NJ):
            k.ts("dve", ang[:, j, :], jrow[:], th[:, j:j + 1], None, ALU.mult)
        sincos(k, al2, St[:], Ct[:], ang[:], [128, NJ, T], "b")
    gluw = al("gluw", [128, 3, 384], BF16)
    with k.scope(f"s5w{l}") as al2:
        load_w_bf16(c, al2, gluw, c.s5_glu_w[l], 384, nkc=3)
    glub = al("glub", [1, 384]); glubb = al("glubb", [1, 384], BF16); onesb = al("onesb", [1, 128], BF16)
    k.dma("sp", glub[:], c.s5_glu_b[l:l + 1, :])
    k.copy("dve", glubb[:], glub[:])
    k.memset("dve", onesb[:], 1.0)
    gn = al("gn", [128, 384])
    k.dma("sp", gn[:], c.s5_norm_g[l:l + 1, :].partition_broadcast(128))
    hp_re = al("hp_re", [128, NJ]); hp_im = al("hp_im", [128, NJ])
    inis = [al(f"ini{i}", [128, 4]) for i in range(2)]
    uj = [al(f"uj{i}", [32, T]) for i in range(3)]
    u128 = [al(f"u128_{i}", [128, T]) for i in range(2)]
    dbl = {nm: [al(f"{nm}{i}", [128, T]) for i in range(2)]
           for nm in ("xr", "xi", "w1", "w2", "w3", "w4", "xtr", "xti", "gr", "gi")}
    hr = [al(f"hr{i}", [128, T]) for i in range(4)]
    hi = [al(f"hi{i}", [128, T]) for i in range(4)]
    yv = al("yv", [128, T]); yq = al("yq", [128, T]); ysg = al("ysg", [128, T])
    yg = [al(f"yg{i}", [128, T]) for i in range(3)]
    ygb = [al(f"ygb{i}", [128, T], BF16) for i in range(3)]
    sg = al("sg", [128, 384]); oz = al("oz", [128, 384]); junk = al("junk", [128, 384])
    ss = al("ss", [128, 1]); oo = [al(f"oo{i}", [128, 384]) for i in range(2)]
    GC = float(2.0 * np.sqrt(2.0 / np.pi))
    nu = 0
    for n in range(L // T):
        t0 = n * T
        rk = tk("projT", t0, t0 + T)
        for jj in range(3):
            ub = u128[(n * 3 + jj) % 2]
            k.dma("pool", ub[:], c.projT[C_SU + jj * 128:C_SU + (jj + 1) * 128, t0:t0 + T], reads=rk)
            for j4 in range(4):
                j = jj * 4 + j4
                u_ = uj[nu % 3]
                db = nu % 2
                xr, xi, w1, w2, w3, w4, xtr, xti, gr, gi = (dbl[nm][db] for nm in
                                                             ("xr", "xi", "w1", "w2", "w3", "w4", "xtr", "xti", "gr", "gi"))
                ini = inis[db]
                PX0, PX1 = (P[0], P[1]) if db == 0 else (P[5], P[6])
                nu += 1
                k.dma("act", u_[:], c.projT[C_SU + j * 32:C_SU + (j + 1) * 32, t0:t0 + T], reads=rk)
                k.mm(PX0[:, :T], BBT[0][:, j, :], u_[:])
                k.mm(PX1[:, :T], BBT[1][:, j, :], u_[:])
                k.copy("act", xr[:], PX0[:, :T])
                k.copy("act", xi[:], PX1[:, :T])
                k.tt("pool", w1[:], Ct[:, j, :], xr[:], ALU.mult)
                k.tt("pool", w2[:], St[:, j, :], xi[:], ALU.mult)
                k.tt("dve", xtr[:], w1[:], w2[:], ALU.add)
                k.tt("pool", w3[:], Ct[:, j, :], xi[:], ALU.mult)
                k.tt("pool", w4[:], St[:, j, :], xr[:], ALU.mult)
                k.tt("dve", xti[:], w3[:], w4[:], ALU.subtract)
                if n == 0:
                    k.memset("dve", ini[:], 0.0)
                else:
                    k.ts("dve", ini[:, 2:3], hp_re[:, j:j + 1], cth[:, j:j + 1], None, ALU.mult)
                    k.stt("dve", ini[:, 0:1], hp_im[:, j:j + 1], nsth[:, j:j + 1], ini[:, 2:3], ALU.mult, ALU.add)
                    k.ts("dve", ini[:, 3:4], hp_re[:, j:j + 1], sth[:, j:j + 1], None, ALU.mult)
                    k.stt("dve", ini[:, 1:2], hp_im[:, j:j + 1], cth[:, j:j + 1], ini[:, 3:4], ALU.mult, ALU.add)
                mb = mag[:, j:j + 1].to_broadcast([128, T])
                k.scan("dve", gr[:], mb, xtr[:], ini[:, 0:1], ALU.mult, ALU.add, reads=[mag, xtr, ini])
                k.scan("dve", gi[:], mb, xti[:], ini[:, 1:2], ALU.mult, ALU.add, reads=[mag, xti, ini])
                h_r, h_i = hr[j4], hi[j4]
                k.tt("pool", w1[:], Ct[:, j, :], gr[:], ALU.mult)
                k.tt("pool", w2[:], St[:, j, :], gi[:], ALU.mult)
                k.tt("dve", h_r[:], w1[:], w2[:], ALU.subtract)
                k.tt("pool", w3[:], St[:, j, :], gr[:], ALU.mult)
                k.tt("pool", w4[:], Ct[:, j, :], gi[:], ALU.mult)
                k.tt("dve", h_i[:], w3[:], w4[:], ALU.add)
                k.copy("act", hp_re[:, j:j + 1], h_r[:, T - 1:T])
                k.copy("act", hp_im[:, j:j + 1], h_i[:, T - 1:T])
            for j4 in range(4):
                j = jj * 4 + j4
                k.mm(P[2][:, :T], CR[:, j, :], hr[j4][:], start=(j4 == 0), stop=False)
                k.mm(P[2][:, :T], CI[:, j, :], hi[j4][:], start=False, stop=(j4 == 3))
            k.stt("dve", yv[:], ub[:], dcol[:, jj:jj + 1], P[2][:, :T], ALU.mult, ALU.add)
            k.act(yq[:], yv[:], AF.Square)
            k.ts("dve", yq[:], yq[:], 0.044715, 1.0, ALU.mult, ALU.add)
            k.tt("pool", yq[:], yq[:], yv[:], ALU.mult)
            k.act(ysg[:], yq[:], AF.Sigmoid, scale=GC)
            k.tt("dve", yg[jj][:], yv[:], ysg[:], ALU.mult)
            k.copy("act", ygb[jj][:], yg[jj][:])
        for tt in range(T // 128):
            ts_ = slice(tt * 128, (tt + 1) * 128)
            for kc in range(3):
                k.mm(P[3][:, :384], ygb[kc][:, ts_], gluw[:, kc, :], start=(kc == 0), stop=False)
            k.mm(P[3][:, :384], onesb[:], glubb[:], start=False, stop=True)
            for kc in range(3):
                k.tr(P[4][:, kc * 128:(kc + 1) * 128], yg[kc][:, ts_], c.identf[:])
            k.act(sg[:], P[3][:, :384], AF.Sigmoid)
            k.tt("dve", oz[:], P[4][:, :384], sg[:], ALU.mult)
            k.act(junk[:], oz[:], AF.Square, accum_out=ss[:])
            k.rstd(ss[:], 384, EPS)
            o_ = oo[(n * 2 + tt) % 2]
            k.stt("dve", o_[:], oz[:], ss[:, 0:1], gn[:], ALU.mult, ALU.mult)
            tok = t0 + tt * 128
            k.dma("sp", c.mixcat[tok:tok + 128, 640:1024], o_[:], writes=tk("mixs5", tok, tok + 128))


NBIS = 16
NEG = -30000.0


def dsa_setup(c):
    nc = c.nc
    c.dsa_norm_g = c.inp("dsa_norm_g", [2, 256])
    q = np.arange(128)
    caus = np.where(q[None, :] <= q[:, None], 0.0, -1e30).astype(np.float32)
    c.caus_d = nc.inline_tensor(caus, "caus_c").ap()
    c.ident4_d = nc.inline_tensor(np.tile(np.eye(128, dtype=np.float32), (1, 4)), "ident4_c").ap()


def phase_dsa(c, l):
    with c.k.scope(f"dsa{l}") as al:
        _phase_dsa(c, l, al)


def _phase_dsa(c, l, al):
    k, nc = c.k, c.nc
    P = c.P
    NQB = L // 128
    ikb = al("ikb", [32, L], BF16)
    dkb = al("dkb", [65, 4, L], BF16)
    vaug = al("vaug", [128, NQB, 4, 65], BF16)
    scores = al("scores", [128, L])
    mb = al("mb", [128, L], BF16)
    caus = al("caus", [128, 128])
    id4 = al("id4", [128, 512], BF16)
    gn = al("gn", [128, 256])
    k.dma("sp", caus[:], c.caus_d)
    k.dma("sp", gn[:], c.dsa_norm_g[l:l + 1, :].partition_broadcast(128))
    with k.scope(f"dsald{l}") as al2:
        st = [al2(f"st{i}", [64, 4, 512]) for i in range(2)]
        st2 = [al2(f"st2{i}", [32, 512]) for i in range(2)]
        st3 = [al2(f"st3{i}", [128, 256]) for i in range(2)]
        id4f = al2("id4f", [128, 512])
        k.dma("sp", id4f[:], c.ident4_d)
        k.copy("dve", id4[:], id4f[:])
        for g in range(L // 512):
            s_ = st[g % 2]
            rk = tk("projT", g * 512, g * 512 + 512)
            k.dma("sp", s_[:], c.projT[C_DK:C_DK + 256, g * 512:(g + 1) * 512].rearrange("(h d) t -> d h t", d=64), reads=rk)
            k.copy("act", dkb[0:64, :, g * 512:(g + 1) * 512], s_[:])
            s2 = st2[g % 2]
            k.dma("pool", s2[:], c.projT[C_IK:C_IK + 32, g * 512:(g + 1) * 512], reads=rk)
            k.copy("dve", ikb[:, g * 512:(g + 1) * 512], s2[:])
        k.memset("dve", dkb[64:65, :, :], 1.0)
        k.memset("dve", vaug[:, :, :, 64:65], 1.0)
        for t in range(NQB):
            s3 = st3[t % 2]
            k.dma("sp", s3[:], c.proj[t * 128:(t + 1) * 128, C_DV:C_DV + 256], reads=tk("proj", t * 128, t * 128 + 128))
            k.copy("act" if t % 2 == 0 else "dve", vaug[:, t, :, 0:64], s3[:].rearrange("p (h d) -> p h d", h=4))
    ones64 = al("ones64", [64, 128], BF16)
    k.memset("dve", ones64[:], 1.0)
    ksq = [al(f"ksq{i}", [64, 512], BF16) for i in range(2)]
    knmax = al("knmax", [128, 4])
    kntmp = al("kntmp", [128, 4])
    k.memset("dve", knmax[:], 0.0)
    ni = 0
    for g in range(L // 512):
        for h in range(4):
            kq = ksq[ni % 2]
            ni += 1
            k.tt("pool", kq[:], dkb[0:64, h, g * 512:(g + 1) * 512], dkb[0:64, h, g * 512:(g + 1) * 512], ALU.mult)
            k.mm(P[0][:, :512], ones64[:], kq[:])
            k.reduce("dve", kntmp[:, h:h + 1], P[0][:, :512], ALU.max)
        k.tt("dve", knmax[:], knmax[:], kntmp[:], ALU.max)
    k.act(knmax[:], knmax[:], AF.Sqrt)
    iqf = [al(f"iqf{i}", [32, 8, 128]) for i in range(2)]
    iqb = al("iqb", [32, 8, 128], BF16)
    iw = [al(f"iw{i}", [128, 8]) for i in range(2)]
    dqf = [al(f"dqf{i}", [64, 4, 128]) for i in range(2)]
    dqa = al("dqa", [65, 4, 128], BF16)
    dqsq = al("dqsq", [64, 4, 128], BF16)
    qn = al("qn", [128, 4])
    mrow = al("mrow", [128, 4])
    mT = al("mT", [4, 128], BF16)
    rl = [al(f"rl{i}", [128, 512]) for i in range(3)]
    lo = al("lo", [128, 1]); hi = al("hi", [128, 1]); mid = al("mid", [128, 1]); cnt = al("cnt", [128, 1])
    flag = al("flag", [128, 1]); dlt = al("dlt", [128, 1]); ssum = al("ssum", [128, 1])
    Rs = al("Rs", [128, NBIS]); cst = al("cst", [128, NBIS])
    for it in range(NBIS):
        k.memset("dve", cst[:, it:it + 1], float(2.0 ** -(it + 1)))
    pT = [al(f"pT{i}", [128, 512], BF16) for i in range(2)]
    oacc = al("oacc", [128, 4, 65])
    rden = al("rden", [128, 4])
    ov = al("ov", [128, 4, 64])
    ss = al("ss", [128, 1]); junk = al("junk", [128, 256])
    oo = [al(f"oo{i}", [128, 256]) for i in range(2)]
    dqa2 = [dqa, al("dqa_b", [65, 4, 128], BF16)]
    nrl = [0]

    def prep(qb):
        t0 = qb * 128
        n = t0 + 128
        rk = tk("projT", t0, n)
        dqa_ = dqa2[qb % 2]
        iq_ = iqf[qb % 2]
        k.dma("sp", iq_[:], c.projT[C_IQ:C_IQ + 256, t0:n].rearrange("(h d) t -> d h t", d=32), reads=rk)
        k.copy("act", iqb[:], iq_[:])
        iw_ = iw[qb % 2]
        k.dma("pool", iw_[:], c.proj[t0:n, C_IW:C_IW + 8], reads=tk("proj", t0, n))
        dq_ = dqf[qb % 2]
        k.dma("sp", dq_[:], c.projT[C_DQ:C_DQ + 256, t0:n].rearrange("(h d) t -> d h t", d=64), reads=rk)
        k.ts("dve", dqa_[0:64, :, :], dq_[:], 0.125, None, ALU.mult)
        k.tt("pool", dqsq[:], dqa_[0:64, :, :], dqa_[0:64, :, :], ALU.mult)
        for h in range(4):
            k.mm(P[0][:, h:h + 1], dqsq[:, h, :], ones64[:, 0:1])
        k.copy("dve", qn[:], P[0][:, 0:4])
        k.act(qn[:], qn[:], AF.Sqrt)
        k.stt("dve", mrow[:], qn[:], -1.0, knmax[:], ALU.mult, ALU.mult)
        k.tr(P[0][:4, 0:128], mrow[:], c.identf[:])
        k.copy("dve", mT[:], P[0][:4, 0:128])
        for h in range(4):
            k.dma("sp", dqa_[64:65, h, :], mT[h:h + 1, :])

    def scoring_chunks(qb):
        n = qb * 128 + 128
        iw_ = iw[qb % 2]
        items = []
        for kc0 in range(0, n, 512):
            w = min(512, n - kc0)

            def chunk(kc0=kc0, w=w):
                for h in range(8):
                    pb = P[h % 2]
                    k.mm(pb[:, :w], iqb[:, h, :], ikb[:, kc0:kc0 + w])
                    r_ = rl[nrl[0] % 3]
                    nrl[0] += 1
                    if h % 4 == 3:
                        k.ts("dve", r_[:, :w], pb[:, :w], 0.0, None, ALU.max)
                    else:
                        k.act(r_[:, :w], pb[:, :w], AF.Relu)
                    if h == 0:
                        k.ts("dve", scores[:, kc0:kc0 + w], r_[:, :w], iw_[:, 0:1], None, ALU.mult)
                    else:
                        k.stt("dve", scores[:, kc0:kc0 + w], r_[:, :w], iw_[:, h:h + 1], scores[:, kc0:kc0 + w],
                              ALU.mult, ALU.add)
            items.append(chunk)
        return items

    def thresh(qb):
        t0 = qb * 128
        n = t0 + 128
        if qb >= 2:
            k.reduce("dve", hi[:], scores[:, :n], ALU.max)
            k.reduce("dve", lo[:], scores[:, :n], ALU.min)
        k.tt("dve", scores[:, t0:n], scores[:, t0:n], caus[:], ALU.add)
        if qb >= 2:
            k.tt("dve", dlt[:], hi[:], lo[:], ALU.subtract)
            k.ts("dve", Rs[:], cst[:], dlt[:, 0:1], None, ALU.mult)
            nd = ((n // 2 + 127) // 128) * 128
            n_act = n - nd
            for it in range(NBIS):
                k.tt("dve", mid[:], lo[:], Rs[:, it:it + 1], ALU.add)
                k.ts("dve", mb[:, :nd], scores[:, :nd], mid[:, 0:1], None, ALU.is_ge, ALU.add, accum_out=cnt[:],
                     reads=[scores, mid], writes=["mbA", cnt])
                k.act(mb[:, nd:n], scores[:, nd:n], AF.Sign, bias=mid[:, 0:1], scale=-1.0, accum_out=ssum[:],
                      reads=[scores, mid], writes=["mbB", ssum])
                k.stt("dve", flag[:], cnt[:], 2.0, ssum[:], ALU.mult, ALU.subtract)
                k.ts("dve", flag[:], flag[:], float(511 - n_act), Rs[:, it:it + 1], ALU.is_ge, ALU.mult)
                k.tt("dve", lo[:], lo[:], flag[:], ALU.add)
        else:
            k.memset("dve", lo[:], -1e29)
        k.ts("dve", mb[:, :n], scores[:, :n], lo[:, 0:1], NEG, ALU.is_lt, ALU.mult,
             reads=[scores, lo], writes=[mb, "mbA", "mbB"])

    def att_tiles(qb):
        dqa_ = dqa2[qb % 2]
        items = []
        for kt in range(qb + 1):
            def tile(kt=kt):
                pb = P[2 + kt % 2]
                ks = slice(kt * 128, (kt + 1) * 128)
                k.mm(pb[:, :], mb[:, ks], id4[:], start=True, stop=False, reads=[mb, "mbA", "mbB", id4])
                for h in range(4):
                    k.mm(pb[:, h * 128:(h + 1) * 128], dkb[:, h, ks], dqa_[:, h, :], start=False, stop=(h == 3))
                p_ = pT[kt % 2]
                k.act(p_[:], pb[:, :], AF.Exp)
                for h in range(4):
                    k.mm(P[4 + h][:, :65], p_[:, h * 128:(h + 1) * 128], vaug[:, kt, h, :], start=(kt == 0), stop=(kt == qb))
            items.append(tile)
        return items

    def finalize(qb):
        t0 = qb * 128
        n = t0 + 128
        for h in range(4):
            k.copy("act" if h % 2 == 0 else "dve", oacc[:, h, :], P[4 + h][:, :65])
        k.recip("dve", rden[:], oacc[:, :, 64:65].rearrange("p h o -> p (h o)"))
        k.tt("dve", ov[:], oacc[:, :, 0:64], bc_last(rden[:], 64), ALU.mult)
        ovf = ov[:].rearrange("p h d -> p (h d)")
        k.act(junk[:], ovf, AF.Square, accum_out=ss[:])
        k.rstd(ss[:], 256, EPS)
        o_ = oo[qb % 2]
        k.stt("dve", o_[:], ovf, ss[:, 0:1], gn[:], ALU.mult, ALU.mult)
        k.dma("sp", c.mixcat[t0:n, 384:640], o_[:], writes=tk("mixdsa", t0, n))

    for step in range(NQB + 1):
        if step < NQB:
            prep(step)
        sc = scoring_chunks(step) if step < NQB else []
        at = att_tiles(step - 1) if step >= 1 else []
        if sc:
            for i, ch in enumerate(sc):
                ch()
                for tl in at[i * len(at) // len(sc):(i + 1) * len(at) // len(sc)]:
                    tl()
        else:
            for tl in at:
                tl()
        if step >= 1:
            finalize(step - 1)
        if step < NQB:
            thresh(step)


NB = (2 * L) // 128 + 32


def w_setup(c):
    nc = c.nc
    k = c.k
    c.w_out = c.inp("w_out", [2, D, D])
    c.router_grp_w = c.inp("router_grp_w", [2, D, 4])
    c.router_grp_b = c.inp("router_grp_b", [2, 4])
    c.router_exp_w = c.inp("router_exp_w", [2, D, 32])
    c.router_exp_b = c.inp("router_exp_b", [2, 32])
    c.xmid = c.scr("xmid", [L, D])
    c.h2b = c.scr("h2b", [L, D], BF16)
    c.rtE = k.sb("rtE", [128, NT, 2, 32], BF16)
    c.rtw = k.sb("rtw", [128, NT, 2])
    c.iota32_d = nc.inline_tensor(np.tile(np.arange(32, dtype=np.float32)[None, :], (128, 1)), "iota32_c").ap()


def phase_w(c, l, xsrc):
    with c.k.scope(f"pw{l}") as al:
        _phase_w(c, l, xsrc, al)


def _phase_w(c, l, xsrc, al):
    k, nc = c.k, c.nc
    P = c.P
    wob = al("wob", [128, 8, D], BF16)
    with k.scope(f"pw{l}w") as al2:
        load_w_bf16(c, al2, wob, c.w_out[l], D)
    wr = al("wr", [128, 8, 36])
    k.dma("sp", wr[:, :, 0:4], c.router_grp_w[l].rearrange("(kc p) n -> p kc n", p=128), sync=True)
    k.dma("sp", wr[:, :, 4:36], c.router_exp_w[l].rearrange("(kc p) n -> p kc n", p=128), sync=True)
    rb = al("rb", [1, 36]); ones1 = al("ones1", [1, 128])
    k.dma("sp", rb[:, 0:4], c.router_grp_b[l:l + 1, :])
    k.dma("sp", rb[:, 4:36], c.router_exp_b[l:l + 1, :])
    k.memset("dve", ones1[:], 1.0)
    GT1 = modchunk(c, al, l, 2, "GT1")[:]
    A2 = modchunk(c, al, l, 4, "A2")[:]
    SH2 = modchunk(c, al, l, 3, "SH2")[:]
    cat = [al(f"cat{i}", [128, D]) for i in range(2)]
    catb = al("catb", [128, D], BF16)
    catT = al("catT", [128, 8, 128], BF16)
    xt = [al(f"xt{i}", [128, D]) for i in range(2)]
    xn = [al(f"xn{i}", [128, D]) for i in range(2)]
    tmp = al("tmp", [128, D])
    h2 = al("h2", [128, D])
    h2bt = [al(f"h2bt{i}", [128, D], BF16) for i in range(2)]
    h2T = al("h2T", [128, 8, 128])
    ss = al("ss", [128, 1]); junk = al("junk", [128, D])
    lg = al("lg", [128, 36])
    gmax = al("gmax", [128, 1]); gsum = al("gsum", [128, 1]); ge = al("ge", [128, 4])
    ohg = al("ohg", [128, 4]); t48 = al("t48", [128, 4, 8]); sel = al("sel", [128, 8]); sel2 = al("sel2", [128, 8])
    m1 = al("m1", [128, 1]); m2 = al("m2", [128, 1]); oh1 = al("oh1", [128, 8]); oh2 = al("oh2", [128, 8])
    wa = al("wa", [128, 1]); wb_ = al("wb_", [128, 1])
    tpb = P[7][:].bitcast(BF16)
    for ti in range(NT):
        t0 = ti * 128
        ct = cat[ti % 2]
        k.dma("act", ct[:], c.mixcat[t0:t0 + 128, :],
              reads=tk("mixgla", t0, t0 + 128) + tk("mixdsa", t0, t0 + 128) + tk("mixs5", t0, t0 + 128))
        x_ = xt[ti % 2]
        k.dma("act", x_[:], xsrc[t0:t0 + 128, :], reads=tk(c.xkey, t0, t0 + 128))
        k.copy("act", catb[:], ct[:])
        for kc in range(8):
            k.tr(tpb[:, kc * 128:(kc + 1) * 128], catb[:, kc * 128:(kc + 1) * 128], c.identb[:])
        k.copy("act", catT[:], tpb.rearrange("p (a b) -> p a b", a=8))
        x_n = xn[ti % 2]
        for half in range(2):
            pb = P[half]
            for kc in range(8):
                k.mm(pb[:, :], catT[:, kc, :], wob[:, kc, half * 512:(half + 1) * 512], start=(kc == 0), stop=(kc == 7))
            hs = slice(half * 512, (half + 1) * 512)
            k.tt("dve", tmp[:, hs], pb[:, :], GT1[:, hs], ALU.mult)
            k.tt("pool", x_n[:, hs], tmp[:, hs], x_[:, hs], ALU.add)
        k.dma("sp", c.xmid[t0:t0 + 128, :], x_n[:], writes=tk("xmid", t0, t0 + 128))
        k.act(junk[:], x_n[:], AF.Square, accum_out=ss[:])
        k.rstd(ss[:], D, EPS)
        k.stt("dve", tmp[:], x_n[:], ss[:, 0:1], A2, ALU.mult, ALU.mult)
        k.tt("pool", h2[:], tmp[:], SH2, ALU.add)
        hb = h2bt[ti % 2]
        k.copy("act", hb[:], h2[:])
        k.dma("sp", c.h2b[t0:t0 + 128, :], hb[:], writes=tk("h2b", t0, t0 + 128))
        for half in range(2):
            for kk in range(4):
                kc = half * 4 + kk
                k.tr(P[2 + half][:, kk * 128:(kk + 1) * 128], h2[:, kc * 128:(kc + 1) * 128], c.identf[:])
            k.copy("act" if half == 0 else "dve", h2T[:, half * 4:(half + 1) * 4, :],
                   P[2 + half][:, :].rearrange("p (a b) -> p a b", a=4))
        for kc in range(8):
            k.mm(P[4][:, :36], h2T[:, kc, :], wr[:, kc, :], start=(kc == 0), stop=False)
        k.mm(P[4][:, :36], ones1[:], rb[:], start=False, stop=True)
        k.copy("dve", lg[:], P[4][:, :36])
        k.reduce("dve", gmax[:], lg[:, 0:4], ALU.max)
        k.ts("dve", ohg[:], lg[:, 0:4], gmax[:, 0:1], None, ALU.is_ge)
        k.ts("dve", ge[:], lg[:, 0:4], gmax[:, 0:1], None, ALU.subtract)
        k.act(ge[:], ge[:], AF.Exp, accum_out=gsum[:])
        k.recip("dve", gsum[:], gsum[:])
        k.tt("dve", t48[:], lg[:, 4:36].rearrange("p (g e) -> p g e", g=4), bc_last(ohg[:], 8), ALU.mult)
        k.reduce("dve", sel[:], t48[:].rearrange("p g e -> p e g"), ALU.add)
        k.reduce("dve", m1[:], sel[:], ALU.max)
        k.ts("dve", oh1[:], sel[:], m1[:, 0:1], None, ALU.is_ge)
        k.stt("dve", sel2[:], oh1[:], -1e30, sel[:], ALU.mult, ALU.add)
        k.reduce("dve", m2[:], sel2[:], ALU.max)
        k.ts("dve", oh2[:], sel2[:], m2[:, 0:1], None, ALU.is_ge)
        k.tt("dve", wa[:], m1[:], m2[:], ALU.subtract)
        k.act(wa[:], wa[:], AF.Sigmoid)
        k.tt("dve", c.rtw[:, ti, 0:1], wa[:], gsum[:], ALU.mult)
        k.tt("dve", c.rtw[:, ti, 1:2], gsum[:], c.rtw[:, ti, 0:1], ALU.subtract)
        k.tt("dve", c.rtE[:, ti, 0, :].rearrange("p (g e) -> p g e", g=4), bc_last(ohg[:], 8), bc_mid(oh1[:], 4), ALU.mult)
        k.tt("dve", c.rtE[:, ti, 1, :].rearrange("p (g e) -> p g e", g=4), bc_last(ohg[:], 8), bc_mid(oh2[:], 4), ALU.mult)


def moe_setup(c):
    nc = c.nc
    k = c.k
    c.exp_w_gate = c.inp("exp_w_gate", [2, 32, D, 512])
    c.exp_w_up = c.inp("exp_w_up", [2, 32, D, 512])
    c.exp_w_down = c.inp("exp_w_down", [2, 32, 512, D])
    c.final_norm_g = c.inp("final_norm_g", [D])
    c.xs = c.scr("xs", [NB * 128, D], BF16)
    c.ys = c.scr("ys", [NB * 128, D])
    c.xnext = c.scr("xnext", [L, D])
    p = np.arange(128)
    c.tris_d = nc.inline_tensor((p[:, None] < p[None, :]).astype(np.float32), "tris_c").ap()
    c.brow_d = nc.inline_tensor(np.tile(np.arange(NB, dtype=np.float32)[None, :], (128, 1)), "brow_c").ap()
    c.bidx_d = nc.inline_tensor((np.arange(8)[None, :] * 128 + p[:, None]).astype(np.float32), "bidx_c").ap()
    c.dest_i = k.sb("dest_i", [128, NT * 2], I32)


def phase_route(c, l):
    with c.k.scope(f"rt{l}") as al:
        _phase_route(c, l, al)


def _phase_route(c, l, al):
    k, nc = c.k, c.nc
    P = c.P
    trisf = al("trisf", [128, 128]); tris = al("tris", [128, 128], BF16); onesb = al("onesb", [128, 128], BF16)
    k.dma("sp", trisf[:], c.tris_d)
    k.copy("dve", tris[:], trisf[:])
    k.memset("dve", onesb[:], 1.0)
    base = al("base", [128, 32]); tmp = al("tmp", [128, 32]); tmp2 = al("tmp2", [128, 32])
    rank = al("rank", [128, NT * 2])
    k.memset("dve", base[:], 0.0)
    i = 0
    for ti in range(NT):
        for k2 in range(2):
            E = c.rtE[:, ti, k2, :]
            pa, pb = P[(i % 2) * 2], P[(i % 2) * 2 + 1]
            k.mm(pa[:, :32], tris[:], E)
            k.mm(pb[:, :32], onesb[:], E)
            k.tt("dve", tmp[:], pa[:, :32], base[:], ALU.add)
            k.tt("dve", tmp2[:], tmp[:], E, ALU.mult)
            k.reduce("dve", rank[:, i:i + 1], tmp2[:], ALU.add)
            k.tt("dve", base[:], pb[:, :32], base[:], ALU.add)
            i += 1
    nblk = al("nblk", [128, 32]); pend = al("pend", [128, 32]); pst = al("pst", [128, 32]); ones32 = al("ones32", [128, 32])
    k.ts("dve", nblk[:], base[:], 1.0 / 128, float(127.0 / 128 - 0.5 + 1.0 / 256), ALU.mult, ALU.add)
    k.ts("dve", nblk[:], nblk[:], 12582912.0, None, ALU.add)
    k.ts("dve", nblk[:], nblk[:], 12582912.0, None, ALU.subtract)
    k.memset("dve", ones32[:], 1.0)
    k.scan("dve", pend[:], ones32[:], nblk[:], 0.0, ALU.mult, ALU.add)
    k.tt("dve", pst[:], pend[:], nblk[:], ALU.subtract)
    k.ts("dve", pst[:], pst[:], 128.0, None, ALU.mult)
    big = al("big", [128, NT * 2, 32])
    destf = al("destf", [128, NT * 2])
    k.tt("dve", big[:], c.rtE[:].rearrange("p a b e -> p (a b) e"), bc_mid(pst[:], NT * 2), ALU.mult)
    k.reduce("dve", destf[:], big[:], ALU.add)
    k.tt("dve", destf[:], destf[:], rank[:], ALU.add)
    k.copy("dve", c.dest_i[:], destf[:])
    brow = al("brow", [128, NB]); big2 = al("big2", [128, NB, 32]); beb = al("beb", [128, NB])
    k.dma("sp", brow[:], c.brow_d)
    k.tt("dve", big2[:], bc_mid(pend[:], NB), bc_last(brow[:], 32), ALU.is_le)
    k.reduce("dve", beb[:], big2[:], ALU.add)
    k.ts("dve", beb[:], beb[:], 31.0, None, ALU.min)
    bidx = al("bidx", [128, 8])
    k.dma("sp", bidx[:], c.bidx_d)
    pen = al("pen", [128, NB])
    k.memset("dve", pen[:, 0:1], 1.0)
    k.tt("dve", pen[:, 1:NB], beb[:, 1:NB], beb[:, 0:NB - 1], ALU.not_equal)
    k.ts("dve", pen[:], pen[:], -1.0e9, 1.0e9, ALU.mult, ALU.add)
    wf = al("wf", [128, NB, 8])
    k.stt("dve", wf[:], bc_last(beb[:], 8), 1024.0, bc_mid(bidx[:], NB), ALU.mult, ALU.add)
    if l > 0:
        k.ts("dve", wf[:], wf[:], float(l * 32 * 1024), None, ALU.add)
    k.tt("dve", wf[:], wf[:], bc_last(pen[:], 8), ALU.add)
    k.copy("dve", c.widx[:], wf[:])
    k.stt("dve", wf[:, :, 0:4], bc_last(beb[:], 4), 512.0, bc_mid(bidx[:, 0:4], NB), ALU.mult, ALU.add)
    if l > 0:
        k.ts("dve", wf[:, :, 0:4], wf[:, :, 0:4], float(l * 32 * 512), None, ALU.add)
    k.tt("dve", wf[:, :, 0:4], wf[:, :, 0:4], bc_last(pen[:], 4), ALU.add)
    k.copy("dve", c.widx_d[:], wf[:, :, 0:4])
    zt = al("zt", [128, D], BF16)
    k.memset("dve", zt[:], 0.0)
    for b in range(NB):
        k.dma("sp" if b % 2 == 0 else "act", c.xs[b * 128:(b + 1) * 128, :], zt[:], writes=[("xsz", b)])
    hb = [al(f"hb{i}", [128, D], BF16) for i in range(2)]
    for ti in range(NT):
        t0 = ti * 128
        h_ = hb[ti % 2]
        k.dma("sp", h_[:], c.h2b[t0:t0 + 128, :], reads=tk("h2b", t0, t0 + 128))
        for k2 in range(2):
            col = ti * 2 + k2
            k.idma(c.xs[:, :], bass.IndirectOffsetOnAxis(ap=c.dest_i[:, col:col + 1], axis=0), h_[:], None,
                   reads=[h_, c.dest_i] + ([("xsz", b) for b in range(NB)] if col == 0 else []),
                   writes=[("xss", col)])


def phase_experts(c, l):
    with c.k.scope(f"ex{l}") as al:
        _phase_experts(c, l, al)


def _phase_experts(c, l, al):
    k, nc = c.k, c.nc
    P = c.P
    wgfs = [al(f"wgf{i}", [128, 8, 512]) for i in range(2)]
    wufs = [al(f"wuf{i}", [128, 8, 512]) for i in range(2)]
    wdfs = [al(f"wdf{i}", [128, 4, D]) for i in range(2)]
    wgb = [al(f"wgb{i}", [128, 8, 512], BF16) for i in range(2)]
    wub = [al(f"wub{i}", [128, 8, 512], BF16) for i in range(2)]
    wdb = [al(f"wdb{i}", [128, 4, D], BF16) for i in range(2)]
    xb = [al(f"xb{i}", [128, D], BF16) for i in range(2)]
    xT = al("xT", [128, 8, 128], BF16)
    sg = al("sg", [128, 512]); hid = al("hid", [128, 512], BF16); hidT = al("hidT", [128, 4, 128], BF16)
    yb = [al(f"yb{i}", [128, D]) for i in range(2)]
    wg_rows = c.exp_w_gate.rearrange("l e k n -> (l e k) n")
    wu_rows = c.exp_w_up.rearrange("l e k n -> (l e k) n")
    wd_rows = c.exp_w_down.rearrange("l e k n -> (l e k) n")
    tpb = P[7][:].bitcast(BF16)
    if not hasattr(c, "bc_regs"):
        c.bc_regs = (nc.gpsimd.to_reg(2 * 32 * 1024 - 1), nc.gpsimd.to_reg(2 * 32 * 512 - 1))
    for b in range(NB):
        wgf, wuf, wdf = wgfs[0], wufs[0], wdfs[0]
        s2 = 0
        for kc in range(8):
            off = bass.IndirectOffsetOnAxis(ap=c.widx[:, b, kc:kc + 1], axis=0)
            k.idma(wgf[:, kc, :], None, wg_rows, off, reads=[c.widx], writes=[("wgf", s2, kc)],
                   bounds_check=c.bc_regs[0], oob_is_err=False)
            k.idma(wuf[:, kc, :], None, wu_rows, off, reads=[c.widx], writes=[("wuf", s2, kc)],
                   bounds_check=c.bc_regs[0], oob_is_err=False)
        for hc in range(4):
            off = bass.IndirectOffsetOnAxis(ap=c.widx_d[:, b, hc:hc + 1], axis=0)
            k.idma(wdf[:, hc, :], None, wd_rows, off, reads=[c.widx_d], writes=[("wdf", s2, hc)],
                   bounds_check=c.bc_regs[1], oob_is_err=False)
        wg_, wu_, wd_ = wgb[b % 2], wub[b % 2], wdb[b % 2]
        k.copy("dve", wg_[:], wgf[:], reads=[("wgf", s2, kc) for kc in range(8)])
        k.copy("act", wu_[:], wuf[:], reads=[("wuf", s2, kc) for kc in range(8)])
        k.copy("dve", wd_[:, 0:2, :], wdf[:, 0:2, :], reads=[("wdf", s2, hc) for hc in range(2)], writes=[("wdb", s2, 0)])
        k.copy("act", wd_[:, 2:4, :], wdf[:, 2:4, :], reads=[("wdf", s2, hc) for hc in range(2, 4)], writes=[("wdb", s2, 1)])
        x_ = xb[b % 2]
        k.dma("act", x_[:], c.xs[b * 128:(b + 1) * 128, :], reads=["xs"])
        for kc in range(8):
            k.tr(tpb[:, kc * 128:(kc + 1) * 128], x_[:, kc * 128:(kc + 1) * 128], c.identb[:])
        k.copy("act", xT[:], tpb.rearrange("p (a b) -> p a b", a=8))
        for kc in range(8):
            k.mm(P[0][:, :], xT[:, kc, :], wg_[:, kc, :], start=(kc == 0), stop=(kc == 7))
        for kc in range(8):
            k.mm(P[1][:, :], xT[:, kc, :], wu_[:, kc, :], start=(kc == 0), stop=(kc == 7))
        k.act(sg[:], P[0][:, :], AF.Silu)
        k.tt("dve", hid[:], sg[:], P[1][:, :], ALU.mult)
        for hc in range(4):
            k.tr(tpb[:, hc * 128:(hc + 1) * 128], hid[:, hc * 128:(hc + 1) * 128], c.identb[:])
        k.copy("act", hidT[:], tpb[:, 0:512].rearrange("p (a b) -> p a b", a=4))
        y_ = yb[b % 2]
        for half in range(2):
            pb = P[2 + half]
            for hc in range(4):
                k.mm(pb[:, :], hidT[:, hc, :], wd_[:, hc, half * 512:(half + 1) * 512], start=(hc == 0), stop=(hc == 3),
                     reads=[hidT, ("wdb", s2, hc // 2)])
            k.copy("act" if half == 0 else "dve", y_[:, half * 512:(half + 1) * 512], pb[:, :])
        k.dma("sp", c.ys[b * 128:(b + 1) * 128, :], y_[:], writes=[("ys", b)])


def phase_combine(c, l, dst, dst_key, final):
    with c.k.scope(f"cb{l}") as al:
        _phase_combine(c, l, dst, dst_key, final, al)


def _phase_combine(c, l, dst, dst_key, final, al):
    k, nc = c.k, c.nc
    GT2 = modchunk(c, al, l, 5, "GT2")[:]
    y0 = [al(f"y0{i}", [128, D]) for i in range(2)]
    y1 = [al(f"y1{i}", [128, D]) for i in range(2)]
    xm = [al(f"xm{i}", [128, D]) for i in range(2)]
    acc = al("acc", [128, D]); xo = [al(f"xo{i}", [128, D]) for i in range(2)]
    ss = al("ss", [128, 1]); junk = al("junk", [128, D])
    if final:
        fg = al("fg", [128, D])
        k.dma("sp", fg[:], c.final_norm_g.rearrange("(o d) -> o d", o=1).partition_broadcast(128))
    for ti in range(NT):
        t0 = ti * 128
        a0, a1, x_ = y0[ti % 2], y1[ti % 2], xm[ti % 2]
        k.idma(a0[:], None, c.ys[:, :], bass.IndirectOffsetOnAxis(ap=c.dest_i[:, 2 * ti:2 * ti + 1], axis=0),
               reads=["ys", c.dest_i], writes=[a0])
        k.idma(a1[:], None, c.ys[:, :], bass.IndirectOffsetOnAxis(ap=c.dest_i[:, 2 * ti + 1:2 * ti + 2], axis=0),
               reads=["ys", c.dest_i], writes=[a1])
        k.dma("act", x_[:], c.xmid[t0:t0 + 128, :], reads=tk("xmid", t0, t0 + 128))
        k.ts("dve", acc[:], a0[:], c.rtw[:, ti, 0:1], None, ALU.mult)
        k.stt("dve", acc[:], a1[:], c.rtw[:, ti, 1:2], acc[:], ALU.mult, ALU.add)
        k.tt("dve", acc[:], acc[:], GT2, ALU.mult)
        o_ = xo[ti % 2]
        k.tt("dve", o_[:], acc[:], x_[:], ALU.add)
        if final:
            k.act(junk[:], o_[:], AF.Square, accum_out=ss[:])
            k.rstd(ss[:], D, EPS)
            k.stt("dve", o_[:], o_[:], ss[:, 0:1], fg[:], ALU.mult, ALU.mult)
        k.dma("sp", dst[t0:t0 + 128, :], o_[:], writes=tk(dst_key, t0, t0 + 128))


def phase_moe(c, l, dst, dst_key, final):
    with c.k.scope(f"moe{l}") as al:
        c.widx = al("widx", [128, NB, 8], I32)
        c.widx_d = al("widx_d", [128, NB, 4], I32)
        phase_route(c, l)
        if os.environ.get("MK_SKIP") != "experts":
            phase_experts(c, l)
    phase_combine(c, l, dst, dst_key, final)


def build_all(nc, nlayers=2):
    c = setup(nc)
    gla_setup(c); s5_setup(c); dsa_setup(c); w_setup(c); moe_setup(c)
    out = nc.dram_tensor("out", [L, D], F32, kind="ExternalOutput").ap()
    xsrc, xkey = c.x, "x"
    for l in range(nlayers):
        adaln(c, l)
        c.xkey = xkey
        phase_a(c, l, xsrc)
        phase_gla(c, l)
        phase_s5(c, l)
        if os.environ.get("MK_SKIP") != "dsa":
            phase_dsa(c, l)
        phase_w(c, l, xsrc)
        last = (l == nlayers - 1)
        phase_moe(c, l, out if last else c.xnext, "out" if last else "xnext", last)
        xsrc, xkey = c.xnext, "xnext"
    c.k.barrier()
    return c


INPUT_NAMES = ["x", "c", "ada_w", "ada_b", "norm1_g", "w_in", "gla_gate_w", "gla_gate_b", "gla_norm_g", "dsa_norm_g",
               "s5_a_re", "s5_a_im", "s5_log_step", "s5_b_re", "s5_b_im", "s5_c_re", "s5_c_im", "s5_d", "s5_glu_w",
               "s5_glu_b", "s5_norm_g", "w_out", "norm2_g", "router_grp_w", "router_grp_b", "router_exp_w",
               "router_exp_b", "exp_w_gate", "exp_w_up", "exp_w_down", "final_norm_g"]


def kernel(**inputs):
    from concourse.bass_utils import run_bass_kernel_spmd
    nc = bass.Bass("TRN2", target_bir_lowering=False)
    build_all(nc)
    n_cores = 4
    in_maps = []
    shared = {n: np.ascontiguousarray(np.asarray(inputs[n], dtype=np.float32)) for n in INPUT_NAMES if n not in ("x", "c")}
    for core in range(n_cores):
        b = core % 4
        d = dict(shared)
        d["x"] = np.ascontiguousarray(np.asarray(inputs["x"][b], dtype=np.float32)[:L])
        d["c"] = np.ascontiguousarray(np.asarray(inputs["c"][b], dtype=np.float32))
        in_maps.append(d)
    res = run_bass_kernel_spmd(nc, in_maps, core_ids=list(range(n_cores)))
    out = np.stack([np.asarray(res.results[b]["out"], dtype=np.float32) for b in range(4)], axis=0)
    return out
```

```python
import os
import numpy as np
import concourse.bass as bass
import concourse.mybir as mybir

F32 = mybir.dt.float32
BF16 = mybir.dt.bfloat16
I32 = mybir.dt.int32
U32 = mybir.dt.uint32
ALU = mybir.AluOpType
AF = mybir.ActivationFunctionType
AX = mybir.AxisListType

SEM_LIMIT = 30000


class KB:
    def __init__(self, nc, n_dma_sems=32, n_eng_sems=6):
        self.nc = nc
        self.E = {"pe": nc.tensor, "dve": nc.vector, "act": nc.scalar, "pool": nc.gpsimd, "sp": nc.sync}
        self.eng_sems = {}
        self.eng_cur = {}
        for e in ("pe", "dve", "act", "pool"):
            self.eng_sems[e] = [nc.alloc_semaphore(f"s_{e}{i}") for i in range(n_eng_sems)]
            self.eng_cur[e] = [0, 0]
        self.dma_pools = {
            "hw": [[nc.alloc_semaphore(f"s_dma{i}"), 0, None] for i in range(n_dma_sems)],
            "sw": [[nc.alloc_semaphore(f"s_swdma{i}"), 0, None] for i in range(40)],
        }
        self.dma_rr = {"hw": 0, "sw": 0}
        self.seen = {e: {} for e in self.E}
        self.last_w = {}
        self.readers = {}
        self.psum_keys = set()
        self.n_ins = 0
        self.n_wait = 0
        self._uid = 0

    def sb(self, name, shape, dt=F32):
        return self.nc.alloc_sbuf_tensor(name, list(shape), dt)

    def barrier(self):
        toks = []
        for e in ("pe", "dve", "act", "pool"):
            cur = self.eng_cur[e]
            if cur[1] > 0:
                toks.append((self.eng_sems[e][cur[0]], cur[1]))
        for pool in self.dma_pools.values():
            for slot in pool:
                if slot[1] > 0:
                    toks.append((slot[0], slot[1]))
        for eng in self.E:
            for sem, val in toks:
                self._wait(eng, sem, val)

    def scope(self, prefix):
        kb = self
        import contextlib

        class _Scope:
            def __enter__(s):
                s.es = contextlib.ExitStack()
                s.es.__enter__()
                return s

            def __call__(s, name, shape, dt=F32):
                kb._uid += 1
                return s.es.enter_context(kb.nc.sbuf_tensor(f"{prefix}_{name}_{kb._uid}", list(shape), dt))

            def __exit__(s, *a):
                if a[0] is None:
                    kb.barrier()
                return s.es.__exit__(*a)
        return _Scope()

    def ps(self, name, shape, dt=F32):
        self.psum_keys.add(name)
        return self.nc.alloc_psum_tensor(name, list(shape), dt)

    @staticmethod
    def key(a):
        if isinstance(a, (str, tuple)):
            return a
        t = getattr(a, "tensor", None)
        return t.name if t is not None else a.name

    def _wait(self, eng, sem, val):
        sid = id(sem)
        d = self.seen[eng]
        if d.get(sid, 0) >= val:
            return
        self.E[eng].wait_ge(sem, val)
        d[sid] = val
        self.n_wait += 1

    def _deps(self, eng, reads, writes):
        deps = {}
        def add(tok):
            if tok is None:
                return
            sem, val, teng = tok
            if teng == "pe" and eng == "pe":
                return
            k = id(sem)
            if k not in deps or deps[k][1] < val:
                deps[k] = (sem, val)
        for r in reads:
            add(self.last_w.get(r))
        for w in writes:
            add(self.last_w.get(w))
            for t in self.readers.get(w, ()):
                add(t)
        for sem, val in deps.values():
            self._wait(eng, sem, val)

    def _commit(self, tok, reads, writes):
        for r in reads:
            self.readers.setdefault(r, []).append(tok)
        for w in writes:
            self.last_w[w] = tok
            self.readers[w] = []

    def op(self, eng, fn, reads, writes):
        reads = [self.key(r) for r in reads if r is not None]
        writes = [self.key(w) for w in writes if w is not None]
        writes = writes + [r for r in reads if r in self.psum_keys and r not in writes]
        self._deps(eng, reads, writes)
        ins = fn(self.E[eng])
        cur = self.eng_cur[eng]
        if cur[1] >= SEM_LIMIT:
            cur[0] += 1
            cur[1] = 0
        cur[1] += 1
        sem = self.eng_sems[eng][cur[0]]
        ins.then_inc(sem, 1)
        self.n_ins += 1
        self._commit((sem, cur[1], eng), reads, writes)
        return ins

    def _dma_slot(self, eng):
        kind = "sw" if eng == "pool" else "hw"
        pool = self.dma_pools[kind]
        slot = pool[self.dma_rr[kind]]
        self.dma_rr[kind] = (self.dma_rr[kind] + 1) % len(pool)
        if slot[1] >= SEM_LIMIT:
            raise RuntimeError("dma sem overflow")
        return slot

    def dma(self, eng, out, in_, reads=None, writes=None, sync=False, **kw):
        if kw.get("allow_slow_non_contiguous"):
            sync = True
        reads = [self.key(r) for r in (reads if reads is not None else [in_])]
        writes = [self.key(w) for w in (writes if writes is not None else [out])]
        slot = self._dma_slot(eng)
        if slot[1] > 0:
            self._wait(eng, slot[0], slot[1])
        self._deps(eng, reads, writes)
        ins = self.E[eng].dma_start(out=out, in_=in_, **kw)
        slot[1] += 16
        ins.then_inc(slot[0], 16)
        self.n_ins += 1
        self._commit((slot[0], slot[1], "dma"), reads, writes)
        if sync:
            self._wait(eng, slot[0], slot[1])
        return ins

    def idma(self, out, out_off, in_, in_off, reads, writes, **kw):
        eng = "pool"
        reads = [self.key(r) for r in reads]
        writes = [self.key(w) for w in writes]
        slot = self._dma_slot(eng)
        if slot[1] > 0:
            self._wait(eng, slot[0], slot[1])
        self._deps(eng, reads, writes)
        ins = self.nc.gpsimd.indirect_dma_start(out=out, out_offset=out_off, in_=in_, in_offset=in_off, **kw)
        slot[1] += 16
        ins.then_inc(slot[0], 16)
        self.n_ins += 1
        self._commit((slot[0], slot[1], "dma"), reads, writes)
        return ins

    def finish(self, keys):
        for k in keys:
            tok = self.last_w.get(self.key(k))
            if tok is not None:
                self._wait("sp", tok[0], tok[1])

    def mm(self, out, lhsT, rhs, start=True, stop=True, reads=None, writes=None):
        r = reads if reads is not None else [lhsT, rhs]
        w = writes if writes is not None else [out]
        return self.op("pe", lambda e: e.matmul(out, lhsT, rhs, start=start, stop=stop), r, w)

    def tr(self, out, in_, ident, reads=None, writes=None):
        r = reads if reads is not None else [in_, ident]
        w = writes if writes is not None else [out]
        return self.op("pe", lambda e: e.transpose(out, in_, ident), r, w)

    def act(self, out, in_, func, bias=None, scale=None, accum_out=None, eng="act", reads=None, writes=None):
        kw = {}
        r = [in_]
        if bias is not None:
            kw["bias"] = bias
            if not isinstance(bias, (int, float)):
                r.append(bias)
        if scale is not None:
            kw["scale"] = scale
            if not isinstance(scale, (int, float)):
                r.append(scale)
        w = [out]
        if accum_out is not None:
            kw["accum_out"] = accum_out
            w.append(accum_out)
        if reads is not None:
            r = reads
        if writes is not None:
            w = writes
        return self.op("act", lambda e: e.activation(out, in_, func, **kw), r, w)

    def ts(self, eng, out, in0, s1, s2=None, op0=ALU.mult, op1=None, accum_out=None, reads=None, writes=None):
        r = [in0]
        if not isinstance(s1, (int, float)):
            r.append(s1)
        if s2 is not None and not isinstance(s2, (int, float)):
            r.append(s2)
        w = [out]
        kw = {}
        if op1 is not None:
            kw["op1"] = op1
        if accum_out is not None:
            kw["accum_out"] = accum_out
            w.append(accum_out)
        if reads is not None:
            r = reads
        if writes is not None:
            w = writes
        return self.op(eng, lambda e: e.tensor_scalar(out, in0, s1, s2, op0, **kw), r, w)

    def tt(self, eng, out, in0, in1, op, reads=None, writes=None):
        r = reads if reads is not None else [in0, in1]
        w = writes if writes is not None else [out]
        return self.op(eng, lambda e: e.tensor_tensor(out, in0, in1, op), r, w)

    def stt(self, eng, out, in0, scalar, in1, op0, op1, accum_out=None, reads=None, writes=None):
        r = [in0, in1]
        if not isinstance(scalar, (int, float)):
            r.append(scalar)
        w = [out]
        kw = {}
        if accum_out is not None:
            kw["accum_out"] = accum_out
            w.append(accum_out)
        if reads is not None:
            r = reads
        if writes is not None:
            w = writes
        return self.op(eng, lambda e: e.scalar_tensor_tensor(out, in0, scalar, in1, op0, op1, **kw), r, w)

    def copy(self, eng, out, in_, reads=None, writes=None):
        r = reads if reads is not None else [in_]
        w = writes if writes is not None else [out]
        if eng == "act":
            return self.op(eng, lambda e: e.copy(out, in_), r, w)
        return self.op(eng, lambda e: e.tensor_copy(out, in_), r, w)

    def memset(self, eng, out, val, writes=None):
        w = writes if writes is not None else [out]
        return self.op(eng, lambda e: e.memset(out, val), [], w)

    def reduce(self, eng, out, in_, op, axis=AX.X, reads=None, writes=None):
        r = reads if reads is not None else [in_]
        w = writes if writes is not None else [out]
        return self.op(eng, lambda e: e.tensor_reduce(out, in_, axis, op), r, w)

    def scan(self, eng, out, d0, d1, init, op0, op1, reads=None, writes=None):
        r = [d0, d1]
        if not isinstance(init, (int, float)):
            r.append(init)
        if reads is not None:
            r = reads
        w = writes if writes is not None else [out]
        return self.op(eng, lambda e: e.tensor_tensor_scan(out, d0, d1, init, op0, op1), r, w)

    def recip(self, eng, out, in_):
        return self.op(eng, lambda e: e.reciprocal(out, in_), [in_], [out])

    def rstd(self, ss, n, eps):
        self.ts("dve", ss, ss, 1.0 / n, eps, ALU.mult, ALU.add)
        self.act(ss, ss, AF.Sqrt)
        self.recip("dve", ss, ss)


L = int(os.environ.get('MK_L', 8192))
D = 1024
DIN = 2616
NT = L // 128
EPS = 1e-6
GSTOP = int(os.environ.get('GSTOP', 99))

C_GQ, C_GK, C_GV, C_GLR, C_GR = 0, 192, 384, 768, 784
C_DQ, C_DK, C_DV, C_IQ, C_IK, C_IW, C_SU = 1168, 1424, 1680, 1936, 2192, 2224, 2232

FM_PIECES = [(0, 128), (128, 256), (256, 384), (768, 784),
             (1168, 1296), (1296, 1424), (1424, 1552), (1552, 1680),
             (1936, 2064), (2064, 2192), (2192, 2224),
             (2232, 2360), (2360, 2488), (2488, 2616)]
TM_PIECES = [(384, 768), (784, 1168), (1680, 1936), (2224, 2232)]


class Ctx:
    pass


def tk(name, t0, t1):
    return [(name, i) for i in range(t0 // 128, (t1 + 127) // 128)]


def setup(nc, ext_scratch=()):
    c = Ctx()
    c.nc = nc
    k = KB(nc)
    c.k = k

    def inp(name, shape):
        return nc.dram_tensor(name, list(shape), F32, kind="ExternalInput").ap()

    def scr(name, shape, dt=F32):
        kind = "ExternalOutput" if name in ext_scratch else "Internal"
        return nc.dram_tensor(name, list(shape), dt, kind=kind).ap()

    c.inp, c.scr = inp, scr
    c.x = inp("x", [L, D])
    c.c = inp("c", [D])
    c.ada_w = inp("ada_w", [2, D, 6 * D])
    c.ada_b = inp("ada_b", [2, 6 * D])
    c.norm1_g = inp("norm1_g", [2, D])
    c.norm2_g = inp("norm2_g", [2, D])
    c.w_in = inp("w_in", [2, D, DIN])
    c.modrow = scr("modrow", [2, 6 * D])
    c.proj = scr("proj", [L, DIN])
    c.projT = scr("projT", [DIN, L])
    ident = np.eye(128, dtype=np.float32)
    c.ident_d = nc.inline_tensor(ident, "ident_c").ap()
    c.identf = k.sb("identf", [128, 128], F32)
    c.identb = k.sb("identb", [128, 128], BF16)
    k.dma("sp", c.identf[:], c.ident_d)
    k.copy("dve", c.identb[:], c.identf[:])
    c.P = [k.ps(f"bank{i}", [128, 512], F32) for i in range(8)]
    return c


def adaln(c, l):
    k, nc = c.k, c.nc
    with k.scope(f"ada{l}") as al:
        _adaln(c, l, al)


def modchunk(c, al, l, idx, name):
    t = al(name, [128, D])
    c.k.dma("sp", t[:], c.modrow[l:l + 1, idx * D:(idx + 1) * D].partition_broadcast(128))
    return t


def _adaln(c, l, al):
    k, nc = c.k, c.nc
    c.condT = al("condT", [128, 8], F32)
    c.adaw = [al(f"adaw{i}", [128, 8, 512], F32) for i in range(2)]
    c.modsb = al("modsb", [1, 6 * D], F32)
    c.adab = al("adab", [1, 6 * D], F32)
    c.gtmp = al("gtmp", [1, D], F32)
    k.dma("sp", c.condT[:], c.c.rearrange("(kc p) -> p kc", p=128), allow_slow_non_contiguous=True)
    k.act(c.condT[:], c.condT[:], AF.Silu)
    k.dma("sp", c.adab[:], c.ada_b[l:l + 1, :])
    for cc in range(12):
        wt = c.adaw[cc % 2]
        k.dma("sp" if cc % 2 == 0 else "pool", wt[:],
              c.ada_w[l, :, cc * 512:(cc + 1) * 512].rearrange("(kc p) n -> p kc n", p=128))
        pb = c.P[cc % 2]
        for kc in range(8):
            k.mm(pb[0:1, :], c.condT[:, kc:kc + 1], wt[:, kc, :], start=(kc == 0), stop=(kc == 7))
        k.tt("dve", c.modsb[0:1, cc * 512:(cc + 1) * 512], pb[0:1, :], c.adab[0:1, cc * 512:(cc + 1) * 512], ALU.add)
    for (gname, sc_chunk) in (("norm1_g", 1), ("norm2_g", 4)):
        g_ap = getattr(c, gname)
        k.dma("sp", c.gtmp[:], g_ap[l:l + 1, :])
        sl = c.modsb[0:1, sc_chunk * D:(sc_chunk + 1) * D]
        k.stt("dve", sl, sl, 1.0, c.gtmp[:], ALU.add, ALU.mult)
    k.dma("sp", c.modrow[l:l + 1, :], c.modsb[:])


def load_w_bf16(c, al, dst, src_ap, ncols, nkc=8):
    k = c.k
    tmps = [al(f"wtmp{i}", [128, ncols], F32) for i in range(2)]
    for kc in range(nkc):
        t = tmps[kc % 2]
        k.dma("sp" if kc % 2 == 0 else "pool", t[:, :ncols], src_ap[kc * 128:(kc + 1) * 128, :])
        k.copy("dve" if kc % 2 == 0 else "pool", dst[:, kc, :], t[:, :ncols])


def phase_a(c, l, xsrc):
    with c.k.scope(f"pa{l}") as al:
        _phase_a(c, l, xsrc, al)


def _phase_a(c, l, xsrc, al):
    k, nc = c.k, c.nc
    c.wbf = al("wbf", [128, 8, DIN], BF16)
    c.xt = [al(f"xt{i}", [128, D], F32) for i in range(2)]
    c.hn = al("hn", [128, D], F32)
    c.hb = [al(f"hb{i}", [128, D], BF16) for i in range(2)]
    c.hT = [al(f"hT{i}", [128, 8, 512], BF16) for i in range(2)]
    c.ss = al("ss", [128, 4], F32)
    c.junk = al("junk", [128, D], F32)
    c.stm = [al(f"stm{i}", [128, 1032], F32) for i in range(2)]
    c.sfm = [al(f"sfm{i}", [128, 512], F32) for i in range(3)]
    with k.scope(f"pa{l}w") as al2:
        load_w_bf16(c, al2, c.wbf, c.w_in[l], DIN)
    A1 = modchunk(c, al, l, 1, "A1")[:]
    SH1 = modchunk(c, al, l, 0, "SH1")[:]
    tp = c.P[7][:].bitcast(BF16)
    nfm = 0
    for g in range(L // 512):
        hT = c.hT[g % 2]
        for t in range(4):
            ti = g * 4 + t
            xt = c.xt[ti % 2]
            hb = c.hb[ti % 2]
            k.dma("act", xt[:], xsrc[ti * 128:(ti + 1) * 128, :])
            ssc = c.ss[:, t:t + 1]
            k.act(c.junk[:], xt[:], AF.Square, accum_out=ssc)
            k.rstd(ssc, D, EPS)
            k.stt("dve", c.hn[:], xt[:], ssc, A1, ALU.mult, ALU.mult)
            k.tt("pool", hb[:], c.hn[:], SH1, ALU.add)
            for kc in range(8):
                k.tr(tp[:, kc * 128:(kc + 1) * 128], hb[:, kc * 128:(kc + 1) * 128], c.identb[:])
            k.copy("act", hT[:, :, t * 128:(t + 1) * 128], tp.rearrange("p (a b) -> p a b", a=8))
            stm = c.stm[ti % 2]
            off = 0
            for pi, (c0, c1) in enumerate(TM_PIECES):
                w = c1 - c0
                pb = c.P[pi % 2]
                for kc in range(8):
                    k.mm(pb[:, :w], hT[:, kc, t * 128:(t + 1) * 128], c.wbf[:, kc, c0:c1], start=(kc == 0), stop=(kc == 7))
                k.copy("act" if pi % 2 == 0 else "dve", stm[:, off:off + w], pb[:, :w])
                off += w
            off = 0
            for (c0, c1) in TM_PIECES:
                w = c1 - c0
                k.dma("sp", c.proj[ti * 128:(ti + 1) * 128, c0:c1], stm[:, off:off + w],
                      writes=tk("proj", ti * 128, ti * 128 + 128))
                off += w
        for pi, (c0, c1) in enumerate(FM_PIECES):
            m = c1 - c0
            pb = c.P[2 + pi % 3]
            sf = c.sfm[nfm % 3]
            nfm += 1
            for kc in range(8):
                k.mm(pb[:m, :], c.wbf[:, kc, c0:c1], hT[:, kc, :], start=(kc == 0), stop=(kc == 7))
            k.copy("act" if pi % 2 == 0 else "dve", sf[:m, :], pb[:m, :])
            k.dma("sp", c.projT[c0:c1, g * 512:(g + 1) * 512], sf[:m, :], writes=tk("projT", g * 512, g * 512 + 512))


def bc_last(ap2, n):
    return ap2.unsqueeze(2).to_broadcast([ap2.shape[0], ap2.shape[1], n])


def bc_mid(ap2, n):
    return ap2.unsqueeze(1).to_broadcast([ap2.shape[0], n, ap2.shape[1]])


def gla_setup(c):
    nc = c.nc
    c.gla_gate_w = c.inp("gla_gate_w", [2, 16, 192])
    c.gla_gate_b = c.inp("gla_gate_b", [2, 192])
    c.gla_norm_g = c.inp("gla_norm_g", [2, 96])
    s = np.arange(128)
    m = ((s[:, None] // 64) == (s[None, :] // 64)) & (s[:, None] <= s[None, :])
    c.tribd_d = nc.inline_tensor((m / 16.0).astype(np.float32), "tribd_c").ap()
    m64 = (s[:64, None] <= s[None, :64]).astype(np.float32)
    c.maskbd_d = nc.inline_tensor(np.tile(m64, (1, 8)), "maskbd_c").ap()
    c.mixcat = c.scr("mixcat", [L, D])


def phase_gla(c, l):
    with c.k.scope(f"gla{l}") as al:
        _phase_gla(c, l, al)


def _phase_gla(c, l, al):
    k, nc = c.k, c.nc
    P = c.P
    tribd = al("tribd", [128, 128])
    maskbd = al("maskbd", [64, 512])
    gw = al("gw", [17, 192])
    gnorm = al("gnorm", [64, 96])
    lrT = [al(f"lrT{i}", [17, 128]) for i in range(2)]
    g16_2 = [al(f"g16{i}", [128, 192]) for i in range(2)]
    ta_2 = [al(f"ta{i}", [128, 192]) for i in range(2)]
    tb_2 = [al(f"tb{i}", [128, 192]) for i in range(2)]
    Eq_2 = [al(f"Eq{i}", [48, 4, 128]) for i in range(2)]
    Ek_2 = [al(f"Ek{i}", [48, 4, 128]) for i in range(2)]
    qk = [al(f"qk{i}", [48, 8, 128]) for i in range(2)]
    qd_2 = [al(f"qd{i}", [48, 4, 128], BF16) for i in range(2)]
    kd_2 = [al(f"kd{i}", [48, 4, 128], BF16) for i in range(2)]
    k2T_2 = [al(f"k2T{i}", [48, 4, 128]) for i in range(2)]
    k2b_2 = [al(f"k2b{i}", [64, 8, 48], BF16) for i in range(2)]
    vr = [al(f"vr{i}", [64, 2, 768]) for i in range(2)]
    vb_2 = [al(f"vb{i}", [64, 2, 384], BF16) for i in range(2)]
    attb_2 = [al(f"attb{i}", [64, 8, 64], BF16) for i in range(2)]
    S = al("S", [48, 4, 96])
    Sb = [al(f"Sb{i}", [48, 4, 96], BF16) for i in range(2)]
    stmp = al("stmp", [48, 4, 96])
    osq = al("osq", [64, 4, 96])
    ss = al("ss", [64, 4])
    on = al("on", [64, 4, 96])
    sr = al("sr", [64, 384])
    oo = [al(f"oo{i}", [64, 2, 384]) for i in range(2)]

    k.dma("sp", tribd[:], c.tribd_d)
    k.dma("sp", maskbd[:], c.maskbd_d)
    k.dma("sp", gw[0:1, :], c.gla_gate_b[l:l + 1, :])
    k.dma("sp", gw[1:17, :], c.gla_gate_w[l])
    k.dma("sp", gnorm[:], c.gla_norm_g[l:l + 1, :].partition_broadcast(64))
    for i in range(2):
        k.memset("dve", lrT[i][0:1, :], 1.0)
    k.memset("dve", S[:], 0.0)
    k.memset("dve", Sb[0][:], 0.0)
    scur = 0
    maskv = maskbd[:].rearrange("p (a c) -> p a c", a=8)
    for ti in range(NT):
        t0 = ti * 128
        g16, ta, tb, Eq, Ek, k2T, qd, kd, k2b, vb, attb = (x_[ti % 2] for x_ in
            (g16_2, ta_2, tb_2, Eq_2, Ek_2, k2T_2, qd_2, kd_2, k2b_2, vb_2, attb_2))
        lt = lrT[ti % 2]
        k.dma("act", lt[1:17, :], c.projT[C_GLR:C_GLR + 16, t0:t0 + 128], reads=tk("projT", t0, t0 + 128))
        q_k = qk[ti % 2]
        k.dma("act", q_k[:], c.projT[0:384, t0:t0 + 128].rearrange("(h d) t -> d h t", d=48),
              reads=tk("projT", t0, t0 + 128))
        v_r = vr[ti % 2]
        k.dma("pool", v_r[:, :, 0:384], c.proj[t0:t0 + 128, C_GV:C_GV + 384].rearrange("(ch p) e -> p ch e", p=64),
              reads=tk("proj", t0, t0 + 128))
        k.dma("pool", v_r[:, :, 384:768], c.proj[t0:t0 + 128, C_GR:C_GR + 384].rearrange("(ch p) e -> p ch e", p=64),
              reads=tk("proj", t0, t0 + 128))
        k.mm(P[0][:, :192], lt[:], gw[:])
        z = P[0][:, :192]
        k.ts("dve", tb[:], z, 0.0, None, ALU.min)
        k.stt("dve", ta[:], tb[:], -2.0, z, ALU.mult, ALU.add)
        k.act(ta[:], ta[:], AF.Exp, scale=-1.0)
        k.act(ta[:], ta[:], AF.Ln, bias=1.0)
        k.tt("dve", g16[:], tb[:], ta[:], ALU.subtract)
        for h in range(4):
            k.mm(P[1][:48, h * 128:(h + 1) * 128], g16[:, h * 48:(h + 1) * 48], tribd[:])
        bcp = P[1][:48, :].rearrange("p (h c) -> p h c", h=4)
        k.act(Eq[:], bcp, AF.Exp)
        k.act(Ek[:], bcp, AF.Exp, scale=-1.0)
        k.stt("dve", qd[:], q_k[:, 0:4, :], float(48 ** -0.5), Eq[:], ALU.mult, ALU.mult)
        k.tt("pool", kd[:], q_k[:, 4:8, :], Ek[:], ALU.mult)
        for h in range(4):
            for ch in range(2):
                sl = slice(ch * 64, ch * 64 + 64)
                k.stt("dve", k2T[:, h, sl], q_k[:, 4 + h, sl], Eq[:, h, ch * 64 + 63:ch * 64 + 64], Ek[:, h, sl],
                      ALU.mult, ALU.mult)
        k.copy("act", vb[:], v_r[:, :, 0:384])
        for ch in range(2):
            sl = slice(ch * 64, ch * 64 + 64)
            for h in range(4):
                a = ch * 4 + h
                k.mm(P[2][:64, a * 64:(a + 1) * 64], kd[:, h, sl], qd[:, h, sl])
        k.tt("dve", attb[:], P[2][:64, :].rearrange("p (a c) -> p a c", a=8), maskv, ALU.mult)
        for ch in range(2):
            sl = slice(ch * 64, ch * 64 + 64)
            for h in range(4):
                a = ch * 4 + h
                k.tr(P[3][:64, a * 48:(a + 1) * 48], k2T[:, h, sl], c.identf[:48, :48])
        k.copy("act", k2b[:], P[3][:64, :384].rearrange("p (a d) -> p a d", a=8))
        o_ = oo[ti % 2]
        for ch in range(2):
            sl = slice(ch * 64, ch * 64 + 64)
            po = P[4 + ch]
            pov = po[:64, :384].rearrange("p (h e) -> p h e", h=4)
            Sin = Sb[scur]
            for h in range(4):
                a = ch * 4 + h
                k.mm(pov[:, h, :], attb[:, a, :], vb[:, ch, h * 96:(h + 1) * 96], start=True, stop=False)
                k.mm(pov[:, h, :], qd[:, h, sl], Sin[:, h, :], start=False, stop=True)
            puv = P[6][:48, :384].rearrange("p (h e) -> p h e", h=4)
            for h in range(4):
                a = ch * 4 + h
                k.mm(puv[:, h, :], k2b[:, a, :], vb[:, ch, h * 96:(h + 1) * 96])
            dec = Eq[:, :, ch * 64 + 63:ch * 64 + 64].rearrange("p h o -> p (h o)")
            k.tt("dve", stmp[:], S[:], bc_last(dec, 96), ALU.mult)
            k.tt("dve", S[:], stmp[:], puv, ALU.add)
            scur = 1 - scur
            k.copy("act", Sb[scur][:], S[:])
            k.act(osq[:], pov, AF.Square)
            k.reduce("dve", ss[:], osq[:], ALU.add)
            k.rstd(ss[:], 96, EPS)
            k.tt("dve", on[:], pov, bc_last(ss[:], 96), ALU.mult)
            k.tt("pool", on[:], on[:], bc_mid(gnorm[:], 4), ALU.mult)
            k.act(sr[:], v_r[:, ch, 384:768], AF.Silu)
            k.tt("pool", o_[:, ch, :], on[:].rearrange("p h e -> p (h e)"), sr[:], ALU.mult)
        k.dma("sp", c.mixcat[t0:t0 + 128, 0:384].rearrange("(ch p) e -> p ch e", p=64), o_[:],
              writes=tk("mixgla", t0, t0 + 128))


S5T = 512
TWO_PI = float(2 * np.pi)


def s5_setup(c):
    nc = c.nc
    for n, shp in (("s5_a_re", [2, 24, 64]), ("s5_a_im", [2, 24, 64]), ("s5_log_step", [2, 24]),
                   ("s5_b_re", [2, 24, 64, 16]), ("s5_b_im", [2, 24, 64, 16]),
                   ("s5_c_re", [2, 24, 16, 64]), ("s5_c_im", [2, 24, 16, 64]), ("s5_d", [2, 24, 16]),
                   ("s5_glu_w", [2, 384, 384]), ("s5_glu_b", [2, 384]), ("s5_norm_g", [2, 384])):
        setattr(c, n, c.inp(n, shp))
    c.jrow_d = nc.inline_tensor(np.arange(S5T, dtype=np.float32)[None, :], "jrow_c").ap()


def sincos(k, al, S, C, A, shape, tag):
    q = al(f"sc_q{tag}", shape)
    ph = al(f"sc_p{tag}", shape)
    for (out, shift) in ((S, 0.0), (C, float(np.pi / 2))):
        k.ts("dve", q[:], A, shift, 1.0 / TWO_PI, ALU.add, ALU.mult)
        k.ts("dve", q[:], q[:], 12582912.0, None, ALU.add)
        k.ts("dve", q[:], q[:], 12582912.0, None, ALU.subtract)
        k.stt("dve", ph[:], q[:], -TWO_PI, A, ALU.mult, ALU.add)
        k.ts("dve", ph[:], ph[:], shift, 3.1415925, ALU.add, ALU.min)
        k.ts("dve", ph[:], ph[:], -3.1415925, None, ALU.max)
        k.act(out, ph[:], AF.Sin)


def phase_s5(c, l):
    with c.k.scope(f"s5{l}") as al:
        _phase_s5(c, l, al)


def _phase_s5(c, l, al):
    k, nc = c.k, c.nc
    P = c.P
    T = S5T
    NJ = 12
    ar = al("ar", [128, NJ]); ai = al("ai", [128, NJ]); ls = al("ls", [128, NJ])
    k.dma("sp", ar[:], c.s5_a_re[l].rearrange("(j two) p -> (two p) j", two=2), allow_slow_non_contiguous=True)
    k.dma("sp", ai[:], c.s5_a_im[l].rearrange("(j two) p -> (two p) j", two=2), allow_slow_non_contiguous=True)
    lsv = c.s5_log_step[l:l + 1, :].rearrange("o (j two) -> o two j", two=2)
    k.dma("sp", ls[0:64, :], lsv[:, 0, :].partition_broadcast(64), allow_slow_non_contiguous=True)
    k.dma("sp", ls[64:128, :], lsv[:, 1, :].partition_broadcast(64), allow_slow_non_contiguous=True)
    dt = al("dt", [128, NJ]); mag = al("mag", [128, NJ]); th = al("th", [128, NJ])
    cth = al("cth", [128, NJ]); sth = al("sth", [128, NJ]); nsth = al("nsth", [128, NJ])
    k.act(dt[:], ls[:], AF.Exp)
    k.tt("dve", mag[:], ar[:], dt[:], ALU.mult)
    k.act(mag[:], mag[:], AF.Exp)
    k.tt("dve", th[:], ai[:], dt[:], ALU.mult)
    q = al("q0", [128, NJ])
    k.ts("dve", q[:], th[:], 1.0 / TWO_PI, 12582912.0, ALU.mult, ALU.add)
    k.ts("dve", q[:], q[:], 12582912.0, None, ALU.subtract)
    k.stt("dve", th[:], q[:], -TWO_PI, th[:], ALU.mult, ALU.add)
    sincos(k, al, sth[:], cth[:], th[:], [128, NJ], "a")
    k.ts("dve", nsth[:], sth[:], -1.0, None, ALU.mult)
    lre = al("lre", [128, NJ]); lim = al("lim", [128, NJ]); den = al("den", [128, NJ])
    t1 = al("pt1", [128, NJ]); t2 = al("pt2", [128, NJ]); cre = al("cre", [128, NJ]); cim = al("cim", [128, NJ])
    k.tt("dve", lre[:], mag[:], cth[:], ALU.mult)
    k.tt("dve", lim[:], mag[:], sth[:], ALU.mult)
    k.tt("dve", den[:], ar[:], ar[:], ALU.mult)
    k.tt("dve", t1[:], ai[:], ai[:], ALU.mult)
    k.tt("dve", den[:], den[:], t1[:], ALU.add)
    k.recip("dve", den[:], den[:])
    k.ts("dve", lre[:], lre[:], -1.0, None, ALU.add)
    k.tt("dve", t1[:], lre[:], ar[:], ALU.mult)
    k.tt("dve", t2[:], lim[:], ai[:], ALU.mult)
    k.tt("dve", t1[:], t1[:], t2[:], ALU.add)
    k.tt("dve", cre[:], t1[:], den[:], ALU.mult)
    k.tt("dve", t1[:], lim[:], ar[:], ALU.mult)
    k.tt("dve", t2[:], lre[:], ai[:], ALU.mult)
    k.tt("dve", t1[:], t1[:], t2[:], ALU.subtract)
    k.tt("dve", cim[:], t1[:], den[:], ALU.mult)
    br = al("br", [128, NJ, 16]); bi = al("bi", [128, NJ, 16])
    k.dma("sp", br[:], c.s5_b_re[l].rearrange("(j two) p c -> (two p) j c", two=2), sync=True)
    k.dma("sp", bi[:], c.s5_b_im[l].rearrange("(j two) p c -> (two p) j c", two=2), sync=True)
    bbr = al("bbr", [128, NJ, 16]); bbi = al("bbi", [128, NJ, 16]); bt = al("bt", [128, NJ, 16])
    k.tt("dve", bbr[:], br[:], bc_last(cre[:], 16), ALU.mult)
    k.tt("dve", bt[:], bi[:], bc_last(cim[:], 16), ALU.mult)
    k.tt("dve", bbr[:], bbr[:], bt[:], ALU.subtract)
    k.tt("dve", bbi[:], bi[:], bc_last(cre[:], 16), ALU.mult)
    k.tt("dve", bt[:], br[:], bc_last(cim[:], 16), ALU.mult)
    k.tt("dve", bbi[:], bbi[:], bt[:], ALU.add)
    Mre = al("Mre", [128, NJ, 32]); Mim = al("Mim", [128, NJ, 32])
    BBT = [al("BBTre", [32, NJ, 128]), al("BBTim", [32, NJ, 128])]
    for (M, bb, dst) in ((Mre, bbr, BBT[0]), (Mim, bbi, BBT[1])):
        k.memset("dve", M[:], 0.0)
        k.copy("dve", M[0:64, :, 0:16], bb[0:64, :, :])
        k.copy("dve", M[64:128, :, 16:32], bb[64:128, :, :])
        for j4 in range(3):
            for jj in range(4):
                j = j4 * 4 + jj
                k.tr(P[0][:32, jj * 128:(jj + 1) * 128], M[:, j, :], c.identf[:])
            k.copy("act", dst[:, j4 * 4:(j4 + 1) * 4, :], P[0][:32, :].rearrange("p (a b) -> p a b", a=4))
    crt = al("crt", [128, NJ, 16]); cit = al("cit", [128, NJ, 16])
    cld = al("cld", [128, 128])
    for (src, dst) in ((c.s5_c_re, crt), (c.s5_c_im, cit)):
        for (j0, nj) in ((0, 8), (8, 4)):
            rows = nj * 16
            for jl in range(nj):
                for two in range(2):
                    k.dma("sp", cld[jl * 16:(jl + 1) * 16, two * 64:(two + 1) * 64], src[l, 2 * (j0 + jl) + two])
            k.tr(P[0][:, :rows], cld[:rows, :], c.identf[:rows, :rows])
            k.copy("act", dst[:, j0:j0 + nj, :], P[0][:, :rows].rearrange("p (j c) -> p j c", c=16))
    CR = al("CRpad", [128, NJ, 128]); CI = al("CIpad", [128, NJ, 128])
    k.memset("dve", CR[:], 0.0)
    k.memset("pool", CI[:], 0.0)
    for j in range(NJ):
        o0 = 32 * (j % 4)
        k.copy("dve", CR[0:64, j, o0:o0 + 16], crt[0:64, j, :])
        k.copy("dve", CR[64:128, j, o0 + 16:o0 + 32], crt[64:128, j, :])
        k.ts("dve", CI[0:64, j, o0:o0 + 16], cit[0:64, j, :], -1.0, None, ALU.mult)
        k.ts("dve", CI[64:128, j, o0 + 16:o0 + 32], cit[64:128, j, :], -1.0, None, ALU.mult)
    dcol = al("dcol", [128, 3])
    k.dma("sp", dcol[:], c.s5_d[l].rearrange("(jj g8) c -> (g8 c) jj", g8=8), allow_slow_non_contiguous=True)
    jrow = al("jrow", [128, T])
    k.dma("sp", jrow[:], c.jrow_d.partition_broadcast(128))
    Ct = al("Ct", [128, NJ, T]); St = al("St", [128, NJ, T])
    with k.scope(f"s5sc{l}") as al2:
        ang = al2("ang", [128, NJ, T])
        for j in range(NJ):
            k.ts("dve", ang[:, j, :], jrow[:], th[:, j:j + 1], None, ALU.mult)
        sincos(k, al2, St[:], Ct[:], ang[:], [128, NJ, T], "b")
    gluw = al("gluw", [128, 3, 384], BF16)
    with k.scope(f"s5w{l}") as al2:
        load_w_bf16(c, al2, gluw, c.s5_glu_w[l], 384, nkc=3)
    glub = al("glub", [1, 384]); glubb = al("glubb", [1, 384], BF16); onesb = al("onesb", [1, 128], BF16)
    k.dma("sp", glub[:], c.s5_glu_b[l:l + 1, :])
    k.copy("dve", glubb[:], glub[:])
    k.memset("dve", onesb[:], 1.0)
    gn = al("gn", [128, 384])
    k.dma("sp", gn[:], c.s5_norm_g[l:l + 1, :].partition_broadcast(128))
    hp_re = al("hp_re", [128, NJ]); hp_im = al("hp_im", [128, NJ])
    inis = [al(f"ini{i}", [128, 4]) for i in range(2)]
    uj = [al(f"uj{i}", [32, T]) for i in range(3)]
    u128 = [al(f"u128_{i}", [128, T]) for i in range(2)]
    dbl = {nm: [al(f"{nm}{i}", [128, T]) for i in range(2)]
           for nm in ("xr", "xi", "w1", "w2", "w3", "w4", "xtr", "xti", "gr", "gi")}
    hr = [al(f"hr{i}", [128, T]) for i in range(4)]
    hi = [al(f"hi{i}", [128, T]) for i in range(4)]
    yv = al("yv", [128, T]); yq = al("yq", [128, T]); ysg = al("ysg", [128, T])
    yg = [al(f"yg{i}", [128, T]) for i in range(3)]
    ygb = [al(f"ygb{i}", [128, T], BF16) for i in range(3)]
    sg = al("sg", [128, 384]); oz = al("oz", [128, 384]); junk = al("junk", [128, 384])
    ss = al("ss", [128, 1]); oo = [al(f"oo{i}", [128, 384]) for i in range(2)]
    GC = float(2.0 * np.sqrt(2.0 / np.pi))
    nu = 0
    for n in range(L // T):
        t0 = n * T
        rk = tk("projT", t0, t0 + T)
        for jj in range(3):
            ub = u128[(n * 3 + jj) % 2]
            k.dma("pool", ub[:], c.projT[C_SU + jj * 128:C_SU + (jj + 1) * 128, t0:t0 + T], reads=rk)
            for j4 in range(4):
                j = jj * 4 + j4
                u_ = uj[nu % 3]
                db = nu % 2
                xr, xi, w1, w2, w3, w4, xtr, xti, gr, gi = (dbl[nm][db] for nm in
                                                             ("xr", "xi", "w1", "w2", "w3", "w4", "xtr", "xti", "gr", "gi"))
                ini = inis[db]
                PX0, PX1 = (P[0], P[1]) if db == 0 else (P[5], P[6])
                nu += 1
                k.dma("act", u_[:], c.projT[C_SU + j * 32:C_SU + (j + 1) * 32, t0:t0 + T], reads=rk)
                k.mm(PX0[:, :T], BBT[0][:, j, :], u_[:])
                k.mm(PX1[:, :T], BBT[1][:, j, :], u_[:])
                k.copy("act", xr[:], PX0[:, :T])
                k.copy("act", xi[:], PX1[:, :T])
                k.tt("pool", w1[:], Ct[:, j, :], xr[:], ALU.mult)
                k.tt("pool", w2[:], St[:, j, :], xi[:], ALU.mult)
                k.tt("dve", xtr[:], w1[:], w2[:], ALU.add)
                k.tt("pool", w3[:], Ct[:, j, :], xi[:], ALU.mult)
                k.tt("pool", w4[:], St[:, j, :], xr[:], ALU.mult)
                k.tt("dve", xti[:], w3[:], w4[:], ALU.subtract)
                if n == 0:
                    k.memset("dve", ini[:], 0.0)
                else:
                    k.ts("dve", ini[:, 2:3], hp_re[:, j:j + 1], cth[:, j:j + 1], None, ALU.mult)
                    k.stt("dve", ini[:, 0:1], hp_im[:, j:j + 1], nsth[:, j:j + 1], ini[:, 2:3], ALU.mult, ALU.add)
                    k.ts("dve", ini[:, 3:4], hp_re[:, j:j + 1], sth[:, j:j + 1], None, ALU.mult)
                    k.stt("dve", ini[:, 1:2], hp_im[:, j:j + 1], cth[:, j:j + 1], ini[:, 3:4], ALU.mult, ALU.add)
                mb = mag[:, j:j + 1].to_broadcast([128, T])
                k.scan("dve", gr[:], mb, xtr[:], ini[:, 0:1], ALU.mult, ALU.add, reads=[mag, xtr, ini])
                k.scan("dve", gi[:], mb, xti[:], ini[:, 1:2], ALU.mult, ALU.add, reads=[mag, xti, ini])
                h_r, h_i = hr[j4], hi[j4]
                k.tt("pool", w1[:], Ct[:, j, :], gr[:], ALU.mult)
                k.tt("pool", w2[:], St[:, j, :], gi[:], ALU.mult)
                k.tt("dve", h_r[:], w1[:], w2[:], ALU.subtract)
                k.tt("pool", w3[:], St[:, j, :], gr[:], ALU.mult)
                k.tt("pool", w4[:], Ct[:, j, :], gi[:], ALU.mult)
                k.tt("dve", h_i[:], w3[:], w4[:], ALU.add)
                k.copy("act", hp_re[:, j:j + 1], h_r[:, T - 1:T])
                k.copy("act", hp_im[:, j:j + 1], h_i[:, T - 1:T])
            for j4 in range(4):
                j = jj * 4 + j4
                k.mm(P[2][:, :T], CR[:, j, :], hr[j4][:], start=(j4 == 0), stop=False)
                k.mm(P[2][:, :T], CI[:, j, :], hi[j4][:], start=False, stop=(j4 == 3))
            k.stt("dve", yv[:], ub[:], dcol[:, jj:jj + 1], P[2][:, :T], ALU.mult, ALU.add)
            k.act(yq[:], yv[:], AF.Square)
            k.ts("dve", yq[:], yq[:], 0.044715, 1.0, ALU.mult, ALU.add)
            k.tt("pool", yq[:], yq[:], yv[:], ALU.mult)
            k.act(ysg[:], yq[:], AF.Sigmoid, scale=GC)
            k.tt("dve", yg[jj][:], yv[:], ysg[:], ALU.mult)
            k.copy("act", ygb[jj][:], yg[jj][:])
        for tt in range(T // 128):
            ts_ = slice(tt * 128, (tt + 1) * 128)
            for kc in range(3):
                k.mm(P[3][:, :384], ygb[kc][:, ts_], gluw[:, kc, :], start=(kc == 0), stop=False)
            k.mm(P[3][:, :384], onesb[:], glubb[:], start=False, stop=True)
            for kc in range(3):
                k.tr(P[4][:, kc * 128:(kc + 1) * 128], yg[kc][:, ts_], c.identf[:])
            k.act(sg[:], P[3][:, :384], AF.Sigmoid)
            k.tt("dve", oz[:], P[4][:, :384], sg[:], ALU.mult)
            k.act(junk[:], oz[:], AF.Square, accum_out=ss[:])
            k.rstd(ss[:], 384, EPS)
            o_ = oo[(n * 2 + tt) % 2]
            k.stt("dve", o_[:], oz[:], ss[:, 0:1], gn[:], ALU.mult, ALU.mult)
            tok = t0 + tt * 128
            k.dma("sp", c.mixcat[tok:tok + 128, 640:1024], o_[:], writes=tk("mixs5", tok, tok + 128))


NBIS = 16
NEG = -30000.0


def dsa_setup(c):
    nc = c.nc
    c.dsa_norm_g = c.inp("dsa_norm_g", [2, 256])
    q = np.arange(128)
    caus = np.where(q[None, :] <= q[:, None], 0.0, -1e30).astype(np.float32)
    c.caus_d = nc.inline_tensor(caus, "caus_c").ap()
    c.ident4_d = nc.inline_tensor(np.tile(np.eye(128, dtype=np.float32), (1, 4)), "ident4_c").ap()


def phase_dsa(c, l):
    with c.k.scope(f"dsa{l}") as al:
        _phase_dsa(c, l, al)


def _phase_dsa(c, l, al):
    k, nc = c.k, c.nc
    P = c.P
    NQB = L // 128
    ikb = al("ikb", [32, L], BF16)
    dkb = al("dkb", [65, 4, L], BF16)
    vaug = al("vaug", [128, NQB, 4, 65], BF16)
    scores = al("scores", [128, L])
    mb = al("mb", [128, L], BF16)
    caus = al("caus", [128, 128])
    id4 = al("id4", [128, 512], BF16)
    gn = al("gn", [128, 256])
    k.dma("sp", caus[:], c.caus_d)
    k.dma("sp", gn[:], c.dsa_norm_g[l:l + 1, :].partition_broadcast(128))
    with k.scope(f"dsald{l}") as al2:
        st = [al2(f"st{i}", [64, 4, 512]) for i in range(2)]
        st2 = [al2(f"st2{i}", [32, 512]) for i in range(2)]
        st3 = [al2(f"st3{i}", [128, 256]) for i in range(2)]
        id4f = al2("id4f", [128, 512])
        k.dma("sp", id4f[:], c.ident4_d)
        k.copy("dve", id4[:], id4f[:])
        for g in range(L // 512):
            s_ = st[g % 2]
            rk = tk("projT", g * 512, g * 512 + 512)
            k.dma("sp", s_[:], c.projT[C_DK:C_DK + 256, g * 512:(g + 1) * 512].rearrange("(h d) t -> d h t", d=64), reads=rk)
            k.copy("act", dkb[0:64, :, g * 512:(g + 1) * 512], s_[:])
            s2 = st2[g % 2]
            k.dma("pool", s2[:], c.projT[C_IK:C_IK + 32, g * 512:(g + 1) * 512], reads=rk)
            k.copy("dve", ikb[:, g * 512:(g + 1) * 512], s2[:])
        k.memset("dve", dkb[64:65, :, :], 1.0)
        k.memset("dve", vaug[:, :, :, 64:65], 1.0)
        for t in range(NQB):
            s3 = st3[t % 2]
            k.dma("sp", s3[:], c.proj[t * 128:(t + 1) * 128, C_DV:C_DV + 256], reads=tk("proj", t * 128, t * 128 + 128))
            k.copy("act" if t % 2 == 0 else "dve", vaug[:, t, :, 0:64], s3[:].rearrange("p (h d) -> p h d", h=4))
    ones64 = al("ones64", [64, 128], BF16)
    k.memset("dve", ones64[:], 1.0)
    ksq = [al(f"ksq{i}", [64, 512], BF16) for i in range(2)]
    knmax = al("knmax", [128, 4])
    kntmp = al("kntmp", [128, 4])
    k.memset("dve", knmax[:], 0.0)
    ni = 0
    for g in range(L // 512):
        for h in range(4):
            kq = ksq[ni % 2]
            ni += 1
            k.tt("pool", kq[:], dkb[0:64, h, g * 512:(g + 1) * 512], dkb[0:64, h, g * 512:(g + 1) * 512], ALU.mult)
            k.mm(P[0][:, :512], ones64[:], kq[:])
            k.reduce("dve", kntmp[:, h:h + 1], P[0][:, :512], ALU.max)
        k.tt("dve", knmax[:], knmax[:], kntmp[:], ALU.max)
    k.act(knmax[:], knmax[:], AF.Sqrt)
    iqf = [al(f"iqf{i}", [32, 8, 128]) for i in range(2)]
    iqb = al("iqb", [32, 8, 128], BF16)
    iw = [al(f"iw{i}", [128, 8]) for i in range(2)]
    dqf = [al(f"dqf{i}", [64, 4, 128]) for i in range(2)]
    dqa = al("dqa", [65, 4, 128], BF16)
    dqsq = al("dqsq", [64, 4, 128], BF16)
    qn = al("qn", [128, 4])
    mrow = al("mrow", [128, 4])
    mT = al("mT", [4, 128], BF16)
    rl = [al(f"rl{i}", [128, 512]) for i in range(3)]
    lo = al("lo", [128, 1]); hi = al("hi", [128, 1]); mid = al("mid", [128, 1]); cnt = al("cnt", [128, 1])
    flag = al("flag", [128, 1]); dlt = al("dlt", [128, 1]); ssum = al("ssum", [128, 1])
    Rs = al("Rs", [128, NBIS]); cst = al("cst", [128, NBIS])
    for it in range(NBIS):
        k.memset("dve", cst[:, it:it + 1], float(2.0 ** -(it + 1)))
    pT = [al(f"pT{i}", [128, 512], BF16) for i in range(2)]
    oacc = al("oacc", [128, 4, 65])
    rden = al("rden", [128, 4])
    ov = al("ov", [128, 4, 64])
    ss = al("ss", [128, 1]); junk = al("junk", [128, 256])
    oo = [al(f"oo{i}", [128, 256]) for i in range(2)]
    dqa2 = [dqa, al("dqa_b", [65, 4, 128], BF16)]
    nrl = [0]

    def prep(qb):
        t0 = qb * 128
        n = t0 + 128
        rk = tk("projT", t0, n)
        dqa_ = dqa2[qb % 2]
        iq_ = iqf[qb % 2]
        k.dma("sp", iq_[:], c.projT[C_IQ:C_IQ + 256, t0:n].rearrange("(h d) t -> d h t", d=32), reads=rk)
        k.copy("act", iqb[:], iq_[:])
        iw_ = iw[qb % 2]
        k.dma("pool", iw_[:], c.proj[t0:n, C_IW:C_IW + 8], reads=tk("proj", t0, n))
        dq_ = dqf[qb % 2]
        k.dma("sp", dq_[:], c.projT[C_DQ:C_DQ + 256, t0:n].rearrange("(h d) t -> d h t", d=64), reads=rk)
        k.ts("dve", dqa_[0:64, :, :], dq_[:], 0.125, None, ALU.mult)
        k.tt("pool", dqsq[:], dqa_[0:64, :, :], dqa_[0:64, :, :], ALU.mult)
        for h in range(4):
            k.mm(P[0][:, h:h + 1], dqsq[:, h, :], ones64[:, 0:1])
        k.copy("dve", qn[:], P[0][:, 0:4])
        k.act(qn[:], qn[:], AF.Sqrt)
        k.stt("dve", mrow[:], qn[:], -1.0, knmax[:], ALU.mult, ALU.mult)
        k.tr(P[0][:4, 0:128], mrow[:], c.identf[:])
        k.copy("dve", mT[:], P[0][:4, 0:128])
        for h in range(4):
            k.dma("sp", dqa_[64:65, h, :], mT[h:h + 1, :])

    def scoring_chunks(qb):
        n = qb * 128 + 128
        iw_ = iw[qb % 2]
        items = []
        for kc0 in range(0, n, 512):
            w = min(512, n - kc0)

            def chunk(kc0=kc0, w=w):
                for h in range(8):
                    pb = P[h % 2]
                    k.mm(pb[:, :w], iqb[:, h, :], ikb[:, kc0:kc0 + w])
                    r_ = rl[nrl[0] % 3]
                    nrl[0] += 1
                    if h % 4 == 3:
                        k.ts("dve", r_[:, :w], pb[:, :w], 0.0, None, ALU.max)
                    else:
                        k.act(r_[:, :w], pb[:, :w], AF.Relu)
                    if h == 0:
                        k.ts("dve", scores[:, kc0:kc0 + w], r_[:, :w], iw_[:, 0:1], None, ALU.mult)
                    else:
                        k.stt("dve", scores[:, kc0:kc0 + w], r_[:, :w], iw_[:, h:h + 1], scores[:, kc0:kc0 + w],
                              ALU.mult, ALU.add)
            items.append(chunk)
        return items

    def thresh(qb):
        t0 = qb * 128
        n = t0 + 128
        if qb >= 2:
            k.reduce("dve", hi[:], scores[:, :n], ALU.max)
            k.reduce("dve", lo[:], scores[:, :n], ALU.min)
        k.tt("dve", scores[:, t0:n], scores[:, t0:n], caus[:], ALU.add)
        if qb >= 2:
            k.tt("dve", dlt[:], hi[:], lo[:], ALU.subtract)
            k.ts("dve", Rs[:], cst[:], dlt[:, 0:1], None, ALU.mult)
            nd = ((n // 2 + 127) // 128) * 128
            n_act = n - nd
            for it in range(NBIS):
                k.tt("dve", mid[:], lo[:], Rs[:, it:it + 1], ALU.add)
                k.ts("dve", mb[:, :nd], scores[:, :nd], mid[:, 0:1], None, ALU.is_ge, ALU.add, accum_out=cnt[:],
                     reads=[scores, mid], writes=["mbA", cnt])
                k.act(mb[:, nd:n], scores[:, nd:n], AF.Sign, bias=mid[:, 0:1], scale=-1.0, accum_out=ssum[:],
                      reads=[scores, mid], writes=["mbB", ssum])
                k.stt("dve", flag[:], cnt[:], 2.0, ssum[:], ALU.mult, ALU.subtract)
                k.ts("dve", flag[:], flag[:], float(511 - n_act), Rs[:, it:it + 1], ALU.is_ge, ALU.mult)
                k.tt("dve", lo[:], lo[:], flag[:], ALU.add)
        else:
            k.memset("dve", lo[:], -1e29)
        k.ts("dve", mb[:, :n], scores[:, :n], lo[:, 0:1], NEG, ALU.is_lt, ALU.mult,
             reads=[scores, lo], writes=[mb, "mbA", "mbB"])

    def att_tiles(qb):
        dqa_ = dqa2[qb % 2]
        items = []
        for kt in range(qb + 1):
            def tile(kt=kt):
                pb = P[2 + kt % 2]
                ks = slice(kt * 128, (kt + 1) * 128)
                k.mm(pb[:, :], mb[:, ks], id4[:], start=True, stop=False, reads=[mb, "mbA", "mbB", id4])
                for h in range(4):
                    k.mm(pb[:, h * 128:(h + 1) * 128], dkb[:, h, ks], dqa_[:, h, :], start=False, stop=(h == 3))
                p_ = pT[kt % 2]
                k.act(p_[:], pb[:, :], AF.Exp)
                for h in range(4):
                    k.mm(P[4 + h][:, :65], p_[:, h * 128:(h + 1) * 128], vaug[:, kt, h, :], start=(kt == 0), stop=(kt == qb))
            items.append(tile)
        return items

    def finalize(qb):
        t0 = qb * 128
        n = t0 + 128
        for h in range(4):
            k.copy("act" if h % 2 == 0 else "dve", oacc[:, h, :], P[4 + h][:, :65])
        k.recip("dve", rden[:], oacc[:, :, 64:65].rearrange("p h o -> p (h o)"))
        k.tt("dve", ov[:], oacc[:, :, 0:64], bc_last(rden[:], 64), ALU.mult)
        ovf = ov[:].rearrange("p h d -> p (h d)")
        k.act(junk[:], ovf, AF.Square, accum_out=ss[:])
        k.rstd(ss[:], 256, EPS)
        o_ = oo[qb % 2]
        k.stt("dve", o_[:], ovf, ss[:, 0:1], gn[:], ALU.mult, ALU.mult)
        k.dma("sp", c.mixcat[t0:n, 384:640], o_[:], writes=tk("mixdsa", t0, n))

    for step in range(NQB + 1):
        if step < NQB:
            prep(step)
        sc = scoring_chunks(step) if step < NQB else []
        at = att_tiles(step - 1) if step >= 1 else []
        if sc:
            for i, ch in enumerate(sc):
                ch()
                for tl in at[i * len(at) // len(sc):(i + 1) * len(at) // len(sc)]:
                    tl()
        else:
            for tl in at:
                tl()
        if step >= 1:
            finalize(step - 1)
        if step < NQB:
            thresh(step)


NB = (2 * L) // 128 + 32


def w_setup(c):
    nc = c.nc
    k = c.k
    c.w_out = c.inp("w_out", [2, D, D])
    c.router_grp_w = c.inp("router_grp_w", [2, D, 4])
    c.router_grp_b = c.inp("router_grp_b", [2, 4])
    c.router_exp_w = c.inp("router_exp_w", [2, D, 32])
    c.router_exp_b = c.inp("router_exp_b", [2, 32])
    c.xmid = c.scr("xmid", [L, D])
    c.h2b = c.scr("h2b", [L, D], BF16)
    c.rtE = k.sb("rtE", [128, NT, 2, 32], BF16)
    c.rtw = k.sb("rtw", [128, NT, 2])
    c.iota32_d = nc.inline_tensor(np.tile(np.arange(32, dtype=np.float32)[None, :], (128, 1)), "iota32_c").ap()


def phase_w(c, l, xsrc):
    with c.k.scope(f"pw{l}") as al:
        _phase_w(c, l, xsrc, al)


def _phase_w(c, l, xsrc, al):
    k, nc = c.k, c.nc
    P = c.P
    wob = al("wob", [128, 8, D], BF16)
    with k.scope(f"pw{l}w") as al2:
        load_w_bf16(c, al2, wob, c.w_out[l], D)
    wr = al("wr", [128, 8, 36])
    k.dma("sp", wr[:, :, 0:4], c.router_grp_w[l].rearrange("(kc p) n -> p kc n", p=128), sync=True)
    k.dma("sp", wr[:, :, 4:36], c.router_exp_w[l].rearrange("(kc p) n -> p kc n", p=128), sync=True)
    rb = al("rb", [1, 36]); ones1 = al("ones1", [1, 128])
    k.dma("sp", rb[:, 0:4], c.router_grp_b[l:l + 1, :])
    k.dma("sp", rb[:, 4:36], c.router_exp_b[l:l + 1, :])
    k.memset("dve", ones1[:], 1.0)
    GT1 = modchunk(c, al, l, 2, "GT1")[:]
    A2 = modchunk(c, al, l, 4, "A2")[:]
    SH2 = modchunk(c, al, l, 3, "SH2")[:]
    cat = [al(f"cat{i}", [128, D]) for i in range(2)]
    catb = al("catb", [128, D], BF16)
    catT = al("catT", [128, 8, 128], BF16)
    xt = [al(f"xt{i}", [128, D]) for i in range(2)]
    xn = [al(f"xn{i}", [128, D]) for i in range(2)]
    tmp = al("tmp", [128, D])
    h2 = al("h2", [128, D])
    h2bt = [al(f"h2bt{i}", [128, D], BF16) for i in range(2)]
    h2T = al("h2T", [128, 8, 128])
    ss = al("ss", [128, 1]); junk = al("junk", [128, D])
    lg = al("lg", [128, 36])
    gmax = al("gmax", [128, 1]); gsum = al("gsum", [128, 1]); ge = al("ge", [128, 4])
    ohg = al("ohg", [128, 4]); t48 = al("t48", [128, 4, 8]); sel = al("sel", [128, 8]); sel2 = al("sel2", [128, 8])
    m1 = al("m1", [128, 1]); m2 = al("m2", [128, 1]); oh1 = al("oh1", [128, 8]); oh2 = al("oh2", [128, 8])
    wa = al("wa", [128, 1]); wb_ = al("wb_", [128, 1])
    tpb = P[7][:].bitcast(BF16)
    for ti in range(NT):
        t0 = ti * 128
        ct = cat[ti % 2]
        k.dma("act", ct[:], c.mixcat[t0:t0 + 128, :],
              reads=tk("mixgla", t0, t0 + 128) + tk("mixdsa", t0, t0 + 128) + tk("mixs5", t0, t0 + 128))
        x_ = xt[ti % 2]
        k.dma("act", x_[:], xsrc[t0:t0 + 128, :], reads=tk(c.xkey, t0, t0 + 128))
        k.copy("act", catb[:], ct[:])
        for kc in range(8):
            k.tr(tpb[:, kc * 128:(kc + 1) * 128], catb[:, kc * 128:(kc + 1) * 128], c.identb[:])
        k.copy("act", catT[:], tpb.rearrange("p (a b) -> p a b", a=8))
        x_n = xn[ti % 2]
        for half in range(2):
            pb = P[half]
            for kc in range(8):
                k.mm(pb[:, :], catT[:, kc, :], wob[:, kc, half * 512:(half + 1) * 512], start=(kc == 0), stop=(kc == 7))
            hs = slice(half * 512, (half + 1) * 512)
            k.tt("dve", tmp[:, hs], pb[:, :], GT1[:, hs], ALU.mult)
            k.tt("pool", x_n[:, hs], tmp[:, hs], x_[:, hs], ALU.add)
        k.dma("sp", c.xmid[t0:t0 + 128, :], x_n[:], writes=tk("xmid", t0, t0 + 128))
        k.act(junk[:], x_n[:], AF.Square, accum_out=ss[:])
        k.rstd(ss[:], D, EPS)
        k.stt("dve", tmp[:], x_n[:], ss[:, 0:1], A2, ALU.mult, ALU.mult)
        k.tt("pool", h2[:], tmp[:], SH2, ALU.add)
        hb = h2bt[ti % 2]
        k.copy("act", hb[:], h2[:])
        k.dma("sp", c.h2b[t0:t0 + 128, :], hb[:], writes=tk("h2b", t0, t0 + 128))
        for half in range(2):
            for kk in range(4):
                kc = half * 4 + kk
                k.tr(P[2 + half][:, kk * 128:(kk + 1) * 128], h2[:, kc * 128:(kc + 1) * 128], c.identf[:])
            k.copy("act" if half == 0 else "dve", h2T[:, half * 4:(half + 1) * 4, :],
                   P[2 + half][:, :].rearrange("p (a b) -> p a b", a=4))
        for kc in range(8):
            k.mm(P[4][:, :36], h2T[:, kc, :], wr[:, kc, :], start=(kc == 0), stop=False)
        k.mm(P[4][:, :36], ones1[:], rb[:], start=False, stop=True)
        k.copy("dve", lg[:], P[4][:, :36])
        k.reduce("dve", gmax[:], lg[:, 0:4], ALU.max)
        k.ts("dve", ohg[:], lg[:, 0:4], gmax[:, 0:1], None, ALU.is_ge)
        k.ts("dve", ge[:], lg[:, 0:4], gmax[:, 0:1], None, ALU.subtract)
        k.act(ge[:], ge[:], AF.Exp, accum_out=gsum[:])
        k.recip("dve", gsum[:], gsum[:])
        k.tt("dve", t48[:], lg[:, 4:36].rearrange("p (g e) -> p g e", g=4), bc_last(ohg[:], 8), ALU.mult)
        k.reduce("dve", sel[:], t48[:].rearrange("p g e -> p e g"), ALU.add)
        k.reduce("dve", m1[:], sel[:], ALU.max)
        k.ts("dve", oh1[:], sel[:], m1[:, 0:1], None, ALU.is_ge)
        k.stt("dve", sel2[:], oh1[:], -1e30, sel[:], ALU.mult, ALU.add)
        k.reduce("dve", m2[:], sel2[:], ALU.max)
        k.ts("dve", oh2[:], sel2[:], m2[:, 0:1], None, ALU.is_ge)
        k.tt("dve", wa[:], m1[:], m2[:], ALU.subtract)
        k.act(wa[:], wa[:], AF.Sigmoid)
        k.tt("dve", c.rtw[:, ti, 0:1], wa[:], gsum[:], ALU.mult)
        k.tt("dve", c.rtw[:, ti, 1:2], gsum[:], c.rtw[:, ti, 0:1], ALU.subtract)
        k.tt("dve", c.rtE[:, ti, 0, :].rearrange("p (g e) -> p g e", g=4), bc_last(ohg[:], 8), bc_mid(oh1[:], 4), ALU.mult)
        k.tt("dve", c.rtE[:, ti, 1, :].rearrange("p (g e) -> p g e", g=4), bc_last(ohg[:], 8), bc_mid(oh2[:], 4), ALU.mult)


def moe_setup(c):
    nc = c.nc
    k = c.k
    c.exp_w_gate = c.inp("exp_w_gate", [2, 32, D, 512])
    c.exp_w_up = c.inp("exp_w_up", [2, 32, D, 512])
    c.exp_w_down = c.inp("exp_w_down", [2, 32, 512, D])
    c.final_norm_g = c.inp("final_norm_g", [D])
    c.xs = c.scr("xs", [NB * 128, D], BF16)
    c.ys = c.scr("ys", [NB * 128, D])
    c.xnext = c.scr("xnext", [L, D])
    p = np.arange(128)
    c.tris_d = nc.inline_tensor((p[:, None] < p[None, :]).astype(np.float32), "tris_c").ap()
    c.brow_d = nc.inline_tensor(np.tile(np.arange(NB, dtype=np.float32)[None, :], (128, 1)), "brow_c").ap()
    c.bidx_d = nc.inline_tensor((np.arange(8)[None, :] * 128 + p[:, None]).astype(np.float32), "bidx_c").ap()
    c.dest_i = k.sb("dest_i", [128, NT * 2], I32)


def phase_route(c, l):
    with c.k.scope(f"rt{l}") as al:
        _phase_route(c, l, al)


def _phase_route(c, l, al):
    k, nc = c.k, c.nc
    P = c.P
    trisf = al("trisf", [128, 128]); tris = al("tris", [128, 128], BF16); onesb = al("onesb", [128, 128], BF16)
    k.dma("sp", trisf[:], c.tris_d)
    k.copy("dve", tris[:], trisf[:])
    k.memset("dve", onesb[:], 1.0)
    base = al("base", [128, 32]); tmp = al("tmp", [128, 32]); tmp2 = al("tmp2", [128, 32])
    rank = al("rank", [128, NT * 2])
    k.memset("dve", base[:], 0.0)
    i = 0
    for ti in range(NT):
        for k2 in range(2):
            E = c.rtE[:, ti, k2, :]
            pa, pb = P[(i % 2) * 2], P[(i % 2) * 2 + 1]
            k.mm(pa[:, :32], tris[:], E)
            k.mm(pb[:, :32], onesb[:], E)
            k.tt("dve", tmp[:], pa[:, :32], base[:], ALU.add)
            k.tt("dve", tmp2[:], tmp[:], E, ALU.mult)
            k.reduce("dve", rank[:, i:i + 1], tmp2[:], ALU.add)
            k.tt("dve", base[:], pb[:, :32], base[:], ALU.add)
            i += 1
    nblk = al("nblk", [128, 32]); pend = al("pend", [128, 32]); pst = al("pst", [128, 32]); ones32 = al("ones32", [128, 32])
    k.ts("dve", nblk[:], base[:], 1.0 / 128, float(127.0 / 128 - 0.5 + 1.0 / 256), ALU.mult, ALU.add)
    k.ts("dve", nblk[:], nblk[:], 12582912.0, None, ALU.add)
    k.ts("dve", nblk[:], nblk[:], 12582912.0, None, ALU.subtract)
    k.memset("dve", ones32[:], 1.0)
    k.scan("dve", pend[:], ones32[:], nblk[:], 0.0, ALU.mult, ALU.add)
    k.tt("dve", pst[:], pend[:], nblk[:], ALU.subtract)
    k.ts("dve", pst[:], pst[:], 128.0, None, ALU.mult)
    big = al("big", [128, NT * 2, 32])
    destf = al("destf", [128, NT * 2])
    k.tt("dve", big[:], c.rtE[:].rearrange("p a b e -> p (a b) e"), bc_mid(pst[:], NT * 2), ALU.mult)
    k.reduce("dve", destf[:], big[:], ALU.add)
    k.tt("dve", destf[:], destf[:], rank[:], ALU.add)
    k.copy("dve", c.dest_i[:], destf[:])
    brow = al("brow", [128, NB]); big2 = al("big2", [128, NB, 32]); beb = al("beb", [128, NB])
    k.dma("sp", brow[:], c.brow_d)
    k.tt("dve", big2[:], bc_mid(pend[:], NB), bc_last(brow[:], 32), ALU.is_le)
    k.reduce("dve", beb[:], big2[:], ALU.add)
    k.ts("dve", beb[:], beb[:], 31.0, None, ALU.min)
    bidx = al("bidx", [128, 8])
    k.dma("sp", bidx[:], c.bidx_d)
    pen = al("pen", [128, NB])
    k.memset("dve", pen[:, 0:1], 1.0)
    k.tt("dve", pen[:, 1:NB], beb[:, 1:NB], beb[:, 0:NB - 1], ALU.not_equal)
    k.ts("dve", pen[:], pen[:], -16384.0, 16384.0, ALU.mult, ALU.add)
    wf = al("wf", [128, NB])
    k.stt("dve", wf[:], beb[:], 128.0, bidx[:, 0:1].to_broadcast([128, NB]), ALU.mult, ALU.add)
    if l > 0:
        k.ts("dve", wf[:], wf[:], float(l * 32 * 128), None, ALU.add)
    k.tt("dve", wf[:], wf[:], pen[:], ALU.add)
    k.copy("dve", c.widx[:], wf[:])
    zt = al("zt", [128, D], BF16)
    k.memset("dve", zt[:], 0.0)
    for b in range(NB):
        k.dma("sp" if b % 2 == 0 else "act", c.xs[b * 128:(b + 1) * 128, :], zt[:], writes=[("xsz", b)])
    hb = [al(f"hb{i}", [128, D], BF16) for i in range(2)]
    for ti in range(NT):
        t0 = ti * 128
        h_ = hb[ti % 2]
        k.dma("sp", h_[:], c.h2b[t0:t0 + 128, :], reads=tk("h2b", t0, t0 + 128))
        for k2 in range(2):
            col = ti * 2 + k2
            k.idma(c.xs[:, :], bass.IndirectOffsetOnAxis(ap=c.dest_i[:, col:col + 1], axis=0), h_[:], None,
                   reads=[h_, c.dest_i] + ([("xsz", b) for b in range(NB)] if col == 0 else []),
                   writes=[("xss", col)])


def phase_experts(c, l):
    with c.k.scope(f"ex{l}") as al:
        _phase_experts(c, l, al)


def _phase_experts(c, l, al):
    k, nc = c.k, c.nc
    P = c.P
    wgfs = [al(f"wgf{i}", [128, 8, 512]) for i in range(2)]
    wufs = [al(f"wuf{i}", [128, 8, 512]) for i in range(2)]
    wdfs = [al(f"wdf{i}", [128, 4, D]) for i in range(2)]
    wgb = [al(f"wgb{i}", [128, 8, 512], BF16) for i in range(2)]
    wub = [al(f"wub{i}", [128, 8, 512], BF16) for i in range(2)]
    wdb = [al(f"wdb{i}", [128, 4, D], BF16) for i in range(2)]
    xb = [al(f"xb{i}", [128, D], BF16) for i in range(2)]
    xT = al("xT", [128, 8, 128], BF16)
    sg = al("sg", [128, 512]); hid = al("hid", [128, 512], BF16); hidT = al("hidT", [128, 4, 128], BF16)
    yb = [al(f"yb{i}", [128, D]) for i in range(2)]
    wg_rows = c.exp_w_gate.rearrange("l e (p j) n -> (l e p) (j n)", j=8)
    wu_rows = c.exp_w_up.rearrange("l e (p j) n -> (l e p) (j n)", j=8)
    wd_rows = c.exp_w_down.rearrange("l e (p j) n -> (l e p) (j n)", j=4)
    tpb = P[7][:].bitcast(BF16)
    if not hasattr(c, "bc_regs"):
        c.bc_regs = (nc.gpsimd.to_reg(2 * 32 * 128 - 1),)
    for b in range(NB):
        wgf, wuf, wdf = wgfs[0], wufs[0], wdfs[0]
        s2 = 0
        off = bass.IndirectOffsetOnAxis(ap=c.widx[:, b:b + 1], axis=0)
        k.idma(wgf[:].rearrange("p a n -> p (a n)"), None, wg_rows, off, reads=[c.widx],
               writes=[("wgf", s2, kc) for kc in range(8)], bounds_check=c.bc_regs[0], oob_is_err=False)
        k.idma(wuf[:].rearrange("p a n -> p (a n)"), None, wu_rows, off, reads=[c.widx],
               writes=[("wuf", s2, kc) for kc in range(8)], bounds_check=c.bc_regs[0], oob_is_err=False)
        k.idma(wdf[:].rearrange("p a n -> p (a n)"), None, wd_rows, off, reads=[c.widx],
               writes=[("wdf", s2, hc) for hc in range(4)], bounds_check=c.bc_regs[0], oob_is_err=False)
        wg_, wu_, wd_ = wgb[b % 2], wub[b % 2], wdb[b % 2]
        k.copy("dve", wg_[:], wgf[:], reads=[("wgf", s2, kc) for kc in range(8)])
        k.copy("act", wu_[:], wuf[:], reads=[("wuf", s2, kc) for kc in range(8)])
        k.copy("dve", wd_[:, 0:2, :], wdf[:, 0:2, :], reads=[("wdf", s2, hc) for hc in range(2)], writes=[("wdb", s2, 0)])
        k.copy("act", wd_[:, 2:4, :], wdf[:, 2:4, :], reads=[("wdf", s2, hc) for hc in range(2, 4)], writes=[("wdb", s2, 1)])
        x_ = xb[b % 2]
        k.dma("act", x_[:], c.xs[b * 128:(b + 1) * 128, :], reads=["xs"])
        for kc in range(8):
            k.tr(tpb[:, kc * 128:(kc + 1) * 128], x_[:].rearrange("r (p j) -> r j p", j=8)[:, kc, :], c.identb[:])
        k.copy("act", xT[:], tpb.rearrange("p (a b) -> p a b", a=8))
        for kc in range(8):
            k.mm(P[0][:, :], xT[:, kc, :], wg_[:, kc, :], start=(kc == 0), stop=(kc == 7))
        for kc in range(8):
            k.mm(P[1][:, :], xT[:, kc, :], wu_[:, kc, :], start=(kc == 0), stop=(kc == 7))
        k.act(sg[:], P[0][:, :], AF.Silu)
        k.tt("dve", hid[:], sg[:], P[1][:, :], ALU.mult)
        for hc in range(4):
            k.tr(tpb[:, hc * 128:(hc + 1) * 128], hid[:].rearrange("r (p j) -> r j p", j=4)[:, hc, :], c.identb[:])
        k.copy("act", hidT[:], tpb[:, 0:512].rearrange("p (a b) -> p a b", a=4))
        y_ = yb[b % 2]
        for half in range(2):
            pb = P[2 + half]
            for hc in range(4):
                k.mm(pb[:, :], hidT[:, hc, :], wd_[:, hc, half * 512:(half + 1) * 512], start=(hc == 0), stop=(hc == 3),
                     reads=[hidT, ("wdb", s2, hc // 2)])
            k.copy("act" if half == 0 else "dve", y_[:, half * 512:(half + 1) * 512], pb[:, :])
        k.dma("sp", c.ys[b * 128:(b + 1) * 128, :], y_[:], writes=[("ys", b)])


def phase_combine(c, l, dst, dst_key, final):
    with c.k.scope(f"cb{l}") as al:
        _phase_combine(c, l, dst, dst_key, final, al)


def _phase_combine(c, l, dst, dst_key, final, al):
    k, nc = c.k, c.nc
    GT2 = modchunk(c, al, l, 5, "GT2")[:]
    y0 = [al(f"y0{i}", [128, D]) for i in range(2)]
    y1 = [al(f"y1{i}", [128, D]) for i in range(2)]
    xm = [al(f"xm{i}", [128, D]) for i in range(2)]
    acc = al("acc", [128, D]); xo = [al(f"xo{i}", [128, D]) for i in range(2)]
    ss = al("ss", [128, 1]); junk = al("junk", [128, D])
    if final:
        fg = al("fg", [128, D])
        k.dma("sp", fg[:], c.final_norm_g.rearrange("(o d) -> o d", o=1).partition_broadcast(128))
    for ti in range(NT):
        t0 = ti * 128
        a0, a1, x_ = y0[ti % 2], y1[ti % 2], xm[ti % 2]
        k.idma(a0[:], None, c.ys[:, :], bass.IndirectOffsetOnAxis(ap=c.dest_i[:, 2 * ti:2 * ti + 1], axis=0),
               reads=["ys", c.dest_i], writes=[a0])
        k.idma(a1[:], None, c.ys[:, :], bass.IndirectOffsetOnAxis(ap=c.dest_i[:, 2 * ti + 1:2 * ti + 2], axis=0),
               reads=["ys", c.dest_i], writes=[a1])
        k.dma("act", x_[:], c.xmid[t0:t0 + 128, :], reads=tk("xmid", t0, t0 + 128))
        k.ts("dve", acc[:], a0[:], c.rtw[:, ti, 0:1], None, ALU.mult)
        k.stt("dve", acc[:], a1[:], c.rtw[:, ti, 1:2], acc[:], ALU.mult, ALU.add)
        k.tt("dve", acc[:], acc[:], GT2, ALU.mult)
        o_ = xo[ti % 2]
        k.tt("dve", o_[:], acc[:], x_[:], ALU.add)
        if final:
            k.act(junk[:], o_[:], AF.Square, accum_out=ss[:])
            k.rstd(ss[:], D, EPS)
            k.stt("dve", o_[:], o_[:], ss[:, 0:1], fg[:], ALU.mult, ALU.mult)
        k.dma("sp", dst[t0:t0 + 128, :], o_[:], writes=tk(dst_key, t0, t0 + 128))


def phase_moe(c, l, dst, dst_key, final):
    with c.k.scope(f"moe{l}") as al:
        c.widx = al("widx", [128, NB], I32)
        phase_route(c, l)
        if os.environ.get("MK_SKIP") != "experts":
            phase_experts(c, l)
    phase_combine(c, l, dst, dst_key, final)


def build_all(nc, nlayers=2):
    c = setup(nc)
    gla_setup(c); s5_setup(c); dsa_setup(c); w_setup(c); moe_setup(c)
    out = nc.dram_tensor("out", [L, D], F32, kind="ExternalOutput").ap()
    xsrc, xkey = c.x, "x"
    for l in range(nlayers):
        adaln(c, l)
        c.xkey = xkey
        phase_a(c, l, xsrc)
        phase_gla(c, l)
        phase_s5(c, l)
        if os.environ.get("MK_SKIP") != "dsa":
            phase_dsa(c, l)
        phase_w(c, l, xsrc)
        last = (l == nlayers - 1)
        phase_moe(c, l, out if last else c.xnext, "out" if last else "xnext", last)
        xsrc, xkey = c.xnext, "xnext"
    c.k.barrier()
    return c


INPUT_NAMES = ["x", "c", "ada_w", "ada_b", "norm1_g", "w_in", "gla_gate_w", "gla_gate_b", "gla_norm_g", "dsa_norm_g",
               "s5_a_re", "s5_a_im", "s5_log_step", "s5_b_re", "s5_b_im", "s5_c_re", "s5_c_im", "s5_d", "s5_glu_w",
               "s5_glu_b", "s5_norm_g", "w_out", "norm2_g", "router_grp_w", "router_grp_b", "router_exp_w",
               "router_exp_b", "exp_w_gate", "exp_w_up", "exp_w_down", "final_norm_g"]


def kernel(**inputs):
    from concourse.bass_utils import run_bass_kernel_spmd
    nc = bass.Bass("TRN2", target_bir_lowering=False)
    build_all(nc)
    n_cores = 4
    in_maps = []
    shared = {n: np.ascontiguousarray(np.asarray(inputs[n], dtype=np.float32)) for n in INPUT_NAMES if n not in ("x", "c")}
    for core in range(n_cores):
        b = core % 4
        d = dict(shared)
        d["x"] = np.ascontiguousarray(np.asarray(inputs["x"][b], dtype=np.float32)[:L])
        d["c"] = np.ascontiguousarray(np.asarray(inputs["c"][b], dtype=np.float32))
        in_maps.append(d)
    res = run_bass_kernel_spmd(nc, in_maps, core_ids=list(range(n_cores)))
    out = np.stack([np.asarray(res.results[b]["out"], dtype=np.float32) for b in range(4)], axis=0)
    return out
```
